# Optimizing a Trainium2 kernel written in Bass

```python
import math
import jax, jax.numpy as jnp
from jax import lax
import numpy as np

D_MODEL = 1024
BATCH = 8
SEQ = 8192
DEPTH = 1
DEC_BATCH = 128
DEC_SEQ = 1
PAST_LEN = 8192
PAGE_SIZE = 128

HEAD_DIM = 64
HA = D_MODEL // (2 * HEAD_DIM)
HB = D_MODEL // (2 * HEAD_DIM)
A_WIDTH = HA * HEAD_DIM
B_WIDTH = HB * HEAD_DIM
MIX_WIDTH = A_WIDTH + B_WIDTH
CONV_K = 4
GDN_CHUNK = 64
ROT_DIM = HEAD_DIM // 4
ROPE_THETA = 500000.0
DILATIONS = ((128, 1), (512, 4), (2048, 16))
MAX_WINDOW = 2048
N_GROUPS = 4
EXPERTS_PER_GROUP = 8
N_EXPERTS = N_GROUPS * EXPERTS_PER_GROUP
TOP_K = 2
D_EXPERT = D_MODEL // 2
MOE_BLOCK = 128
PLE_DIM = 256
NORM_EPS = 1e-6
OFF_Z = 3 * A_WIDTH
OFF_A = OFF_Z + A_WIDTH
OFF_B = OFF_A + HA
OFF_WIN = OFF_B + HA
IN_WIDTH = OFF_WIN + 3 * B_WIDTH

kernel_name = 'hymba_gdn_dilated_hmoe_step'

F32 = jnp.float32


def rmsnorm(x, g):
    xf = x.astype(F32)
    y = xf * lax.rsqrt(jnp.mean(xf * xf, axis=-1, keepdims=True) + NORM_EPS)
    return (y * g.astype(F32)).astype(x.dtype)


def l2norm(x):
    xf = x.astype(F32)
    return xf * lax.rsqrt(jnp.sum(xf * xf, axis=-1, keepdims=True) + NORM_EPS)


def rope_partial(x, pos):
    half = ROT_DIM // 2
    inv = ROPE_THETA ** (-jnp.arange(half, dtype=F32) * (2.0 / ROT_DIM))
    ang = pos.astype(F32)[:, None] * inv[None, :]
    cos = jnp.cos(ang)[None, :, None, :]
    sin = jnp.sin(ang)[None, :, None, :]
    xr = x[..., :ROT_DIM].astype(F32)
    x1, x2 = xr[..., :half], xr[..., half:]
    rot = jnp.concatenate([x1 * cos - x2 * sin, x2 * cos + x1 * sin], axis=-1)
    return jnp.concatenate([rot.astype(x.dtype), x[..., ROT_DIM:]], axis=-1)


def causal_conv(x, prefix, w):
    T = x.shape[1]
    xp = jnp.concatenate([prefix.astype(x.dtype), x], axis=1)
    y = sum(xp[:, j:j + T] * w[j] for j in range(CONV_K))
    return jax.nn.silu(y), xp[:, -(CONV_K - 1):]


def gated_delta_chunked(q, k, v, log_a, beta, s0):
    N, T, H, DK = q.shape
    C = GDN_CHUNK
    Tp = -(-T // C) * C
    nc = Tp // C

    def blocks(t):
        t = jnp.pad(t.astype(F32), [(0, 0), (0, Tp - T)] + [(0, 0)] * (t.ndim - 2))
        t = t.reshape((N, nc, C) + t.shape[2:])
        return jnp.moveaxis(t, 2, 3)

    q = blocks(q) * (DK ** -0.5)
    k = blocks(k)
    v = blocks(v)
    g = jnp.cumsum(blocks(log_a), axis=-1)
    b = blocks(beta)
    incl = jnp.tril(jnp.ones((C, C), bool))
    strict = jnp.tril(jnp.ones((C, C), bool), -1)
    decay = jnp.exp(jnp.where(incl, g[..., :, None] - g[..., None, :], -jnp.inf))
    kb = k * b[..., None]
    A = jnp.where(strict, jnp.einsum('nchid,nchjd->nchij', kb, k) * decay, 0.0)
    eye = jnp.eye(C, dtype=F32)
    Tm = lax.linalg.triangular_solve(eye + A, jnp.broadcast_to(eye, A.shape), left_side=True, lower=True)
    u = Tm @ (v * b[..., None])
    w = Tm @ (kb * jnp.exp(g)[..., None])
    qk = jnp.einsum('nchid,nchjd->nchij', q, k) * decay
    q_dec = q * jnp.exp(g)[..., None]
    k_dec = k * jnp.exp(g[..., -1:] - g)[..., None]
    a_last = jnp.exp(g[..., -1])

    def step(S, xs):
        u_c, w_c, qk_c, qd_c, kd_c, al_c = xs
        v_new = u_c - w_c @ S
        o_c = qd_c @ S + qk_c @ v_new
        S = S * al_c[..., None, None] + jnp.swapaxes(kd_c, -1, -2) @ v_new
        return S, o_c

    xs = tuple(jnp.moveaxis(t, 1, 0) for t in (u, w, qk, q_dec, k_dec, a_last))
    S, o = lax.scan(step, s0.astype(F32), xs)
    o = jnp.swapaxes(jnp.moveaxis(o, 0, 1), 2, 3).reshape(N, Tp, H, -1)[:, :T]
    return o, S


def delta_heads(qkv, z, a_logit, b_logit, conv_prefix, s0, w_conv, a_log, dt_bias, g_out):
    N, T, _ = qkv.shape
    c, conv_new = causal_conv(qkv, conv_prefix, w_conv)
    c = c.reshape(N, T, 3, HA, HEAD_DIM)
    q, k, v = l2norm(c[:, :, 0]), l2norm(c[:, :, 1]), c[:, :, 2]
    log_a = -jnp.exp(a_log.astype(F32)) * jax.nn.softplus(a_logit.astype(F32) + dt_bias.astype(F32))
    beta = jax.nn.sigmoid(b_logit.astype(F32))
    o, s_new = gated_delta_chunked(q, k, v, log_a, beta, s0)
    o = rmsnorm(o, g_out) * jax.nn.silu(z.astype(F32).reshape(N, T, HA, HEAD_DIM))
    return o.reshape(N, T, A_WIDTH).astype(qkv.dtype), conv_new, s_new.astype(s0.dtype)


def window_qkv(qkv, pos):
    N, T, _ = qkv.shape
    qkv = qkv.reshape(N, T, 3, HB, HEAD_DIM)
    return rope_partial(qkv[:, :, 0], pos), rope_partial(qkv[:, :, 1], pos), qkv[:, :, 2]


def dilated_band(q, k, v, window, dil):
    N, S, H, D = q.shape
    band = window // dil
    unit = band * dil
    Sp = -(-S // unit) * unit
    L = Sp // dil
    nb = L // band

    def split(t):
        t = jnp.pad(t, ((0, 0), (0, Sp - S), (0, 0), (0, 0)))
        t = t.reshape(N, L, dil, H, D).transpose(0, 2, 1, 3, 4)
        return t.reshape(N * dil, nb, band, H, D)

    def with_prev(t):
        prev = jnp.concatenate([jnp.zeros_like(t[:, :1]), t[:, :-1]], axis=1)
        return jnp.concatenate([prev, t], axis=2)

    qs = split(q)
    ks = with_prev(split(k))
    vs = with_prev(split(v))
    s = jnp.einsum('mbqhd,mbkhd->mbhqk', qs, ks).astype(F32) * (D ** -0.5)
    a = jnp.arange(band)[:, None]
    c = jnp.arange(2 * band)[None, :]
    dist = band + a - c
    valid = (dist >= 0) & (dist <= band)
    valid = valid[None] & ((jnp.arange(nb)[:, None, None] > 0) | (c[None] >= band))
    s = jnp.where(valid[None, :, None], s, -jnp.inf)
    lse = jax.nn.logsumexp(s, axis=-1)
    p = jnp.exp(s - lse[..., None])
    o = jnp.einsum('mbhqk,mbkhd->mbqhd', p, vs.astype(F32))
    o = o.reshape(N, dil, L, H, D).transpose(0, 2, 1, 3, 4).reshape(N, Sp, H, D)[:, :S]
    lse = jnp.swapaxes(lse, -1, -2).reshape(N, dil, L, H).transpose(0, 2, 1, 3).reshape(N, Sp, H)[:, :S]
    return o, lse


def dilated_gather(q, k_all, v_all, n_past):
    N, T, H, D = q.shape
    qf = q.astype(F32) * (D ** -0.5)
    results = []
    for window, dil in DILATIONS:
        n = window // dil + 1
        idx = n_past + jnp.arange(T)[:, None] - dil * jnp.arange(n)[None, :]
        valid = idx >= 0
        idx = jnp.maximum(idx, 0)
        kg = k_all[:, idx].astype(F32)
        vg = v_all[:, idx].astype(F32)
        s = jnp.einsum('nthd,ntjhd->nthj', qf, kg)
        s = jnp.where(valid[None, :, None, :], s, -jnp.inf)
        lse = jax.nn.logsumexp(s, axis=-1)
        p = jnp.exp(s - lse[..., None])
        results.append((jnp.einsum('nthj,ntjhd->nthd', p, vg), lse))
    return results


def merge_dilations(results, g_out):
    o = jnp.stack([r[0] for r in results])
    lse = jnp.stack([r[1] for r in results])
    wgt = jax.nn.softmax(lse, axis=0)
    o = jnp.sum(wgt[..., None] * o, axis=0)
    N, T = o.shape[:2]
    return rmsnorm(o, g_out).reshape(N, T, B_WIDTH)


def hier_moe(x, w_rg, b_rg, w_re, b_re, w_gate, w_up, w_down):
    Tn, D = x.shape
    g_logits = (x @ w_rg).astype(F32) + b_rg.astype(F32)
    grp = jnp.argmax(g_logits, axis=-1)
    p_grp = jnp.take_along_axis(jax.nn.softmax(g_logits, axis=-1), grp[:, None], axis=-1)
    e_logits = ((x @ w_re.reshape(D, N_EXPERTS)).astype(F32) + b_re.reshape(N_EXPERTS).astype(F32))
    e_logits = e_logits.reshape(Tn, N_GROUPS, EXPERTS_PER_GROUP)
    e_in = jnp.take_along_axis(e_logits, grp[:, None, None], axis=1)[:, 0]
    top_v, top_i = lax.top_k(e_in, TOP_K)
    gate = p_grp * jax.nn.softmax(top_v, axis=-1)
    eid = (grp[:, None] * EXPERTS_PER_GROUP + top_i).reshape(-1)
    M = Tn * TOP_K
    order = jnp.argsort(eid)
    e_sorted = eid[order]
    tok = order // TOP_K
    sizes = jnp.bincount(eid, length=N_EXPERTS)
    padded = (sizes + MOE_BLOCK - 1) // MOE_BLOCK * MOE_BLOCK
    start = jnp.cumsum(sizes) - sizes
    pend = jnp.cumsum(padded)
    pstart = pend - padded
    dest = pstart[e_sorted] + jnp.arange(M) - start[e_sorted]
    n_blk = -(-M // MOE_BLOCK) + N_EXPERTS
    x_buf = jnp.zeros((n_blk * MOE_BLOCK, D), x.dtype).at[dest].set(x[tok])
    blk_expert = jnp.minimum(jnp.searchsorted(pend, jnp.arange(n_blk) * MOE_BLOCK, side='right'), N_EXPERTS - 1)

    def expert_block(args):
        xb, e = args
        hb = jax.nn.silu(xb @ w_gate[e]) * (xb @ w_up[e])
        return hb @ w_down[e]

    y_buf = lax.map(expert_block, (x_buf.reshape(n_blk, MOE_BLOCK, D), blk_expert)).reshape(n_blk * MOE_BLOCK, D)
    contrib = y_buf[dest] * gate.reshape(-1)[order][:, None].astype(x.dtype)
    return jnp.zeros_like(x).at[tok].add(contrib)


def setup_inputs(seed: int = 0) -> dict:
    key = jax.random.key(seed)
    ks = iter(jax.random.split(key, 40))

    def nrm(shape, scale):
        return jax.random.normal(next(ks), shape, F32) * scale

    win_buf = min(MAX_WINDOW, PAST_LEN)
    x_prompt = nrm((BATCH, SEQ, D_MODEL), 1.0)
    x_sample = nrm((DEC_BATCH, DEC_SEQ, D_MODEL), 1.0)
    cache_win_k = nrm((DEPTH, DEC_BATCH, win_buf, HB, HEAD_DIM), 1.0)
    cache_win_v = nrm((DEPTH, DEC_BATCH, win_buf, HB, HEAD_DIM), 1.0)
    state_conv = nrm((DEPTH, DEC_BATCH, CONV_K - 1, 3 * A_WIDTH), 1.0)
    state_delta = nrm((DEPTH, DEC_BATCH, HA, HEAD_DIM, HEAD_DIM), 0.1)
    p_prompt = nrm((DEPTH, BATCH, SEQ, PLE_DIM), 1.0)
    p_sample = nrm((DEPTH, DEC_BATCH, DEC_SEQ, PLE_DIM), 1.0)
    g_attn_norm = 1.0 + nrm((DEPTH, D_MODEL), 0.02)
    w_in = nrm((DEPTH, D_MODEL, IN_WIDTH), D_MODEL ** -0.5)
    w_conv = nrm((DEPTH, CONV_K, 3 * A_WIDTH), CONV_K ** -0.5)
    a_log = jnp.log(jax.random.uniform(next(ks), (DEPTH, HA), F32, 1.0, 16.0))
    dt = jnp.exp(jax.random.uniform(next(ks), (DEPTH, HA), F32, math.log(1e-3), math.log(1e-1)))
    dt_bias = dt + jnp.log(-jnp.expm1(-dt))
    g_a_out = 1.0 + nrm((DEPTH, HEAD_DIM), 0.02)
    g_b_out = 1.0 + nrm((DEPTH, HEAD_DIM), 0.02)
    w_out = nrm((DEPTH, MIX_WIDTH, D_MODEL), MIX_WIDTH ** -0.5)
    g_ffn_norm = 1.0 + nrm((DEPTH, D_MODEL), 0.02)
    w_router_group = nrm((DEPTH, D_MODEL, N_GROUPS), D_MODEL ** -0.5)
    b_router_group = nrm((DEPTH, N_GROUPS), 0.01)
    w_router_expert = nrm((DEPTH, D_MODEL, N_GROUPS, EXPERTS_PER_GROUP), D_MODEL ** -0.5)
    b_router_expert = nrm((DEPTH, N_GROUPS, EXPERTS_PER_GROUP), 0.01)
    w_exp_gate = nrm((DEPTH, N_EXPERTS, D_MODEL, D_EXPERT), D_MODEL ** -0.5)
    w_exp_up = nrm((DEPTH, N_EXPERTS, D_MODEL, D_EXPERT), D_MODEL ** -0.5)
    w_exp_down = nrm((DEPTH, N_EXPERTS, D_EXPERT, D_MODEL), D_EXPERT ** -0.5)
    w_ple_gate = nrm((DEPTH, D_MODEL, D_MODEL), D_MODEL ** -0.5)
    w_ple_proj = nrm((DEPTH, PLE_DIM, D_MODEL), PLE_DIM ** -0.5)
    g_final = 1.0 + nrm((D_MODEL,), 0.02)
    return {'x_prompt': x_prompt, 'x_sample': x_sample, 'cache_win_k': cache_win_k, 'cache_win_v': cache_win_v,
            'state_conv': state_conv, 'state_delta': state_delta, 'p_prompt': p_prompt, 'p_sample': p_sample,
            'g_attn_norm': g_attn_norm, 'w_in': w_in, 'w_conv': w_conv, 'a_log': a_log, 'dt_bias': dt_bias,
            'g_a_out': g_a_out, 'g_b_out': g_b_out, 'w_out': w_out, 'g_ffn_norm': g_ffn_norm,
            'w_router_group': w_router_group, 'b_router_group': b_router_group,
            'w_router_expert': w_router_expert, 'b_router_expert': b_router_expert,
            'w_exp_gate': w_exp_gate, 'w_exp_up': w_exp_up, 'w_exp_down': w_exp_down,
            'w_ple_gate': w_ple_gate, 'w_ple_proj': w_ple_proj, 'g_final': g_final}


def reference(x_prompt, x_sample, cache_win_k, cache_win_v, state_conv, state_delta, p_prompt, p_sample,
              g_attn_norm, w_in, w_conv, a_log, dt_bias, g_a_out, g_b_out, w_out, g_ffn_norm,
              w_router_group, b_router_group, w_router_expert, b_router_expert,
              w_exp_gate, w_exp_up, w_exp_down, w_ple_gate, w_ple_proj, g_final):
    NP, S, _ = x_prompt.shape
    NS, T, _ = x_sample.shape
    n_past = cache_win_k.shape[2]
    pos_p = jnp.arange(S, dtype=jnp.int32)
    pos_s = PAST_LEN + jnp.arange(T, dtype=jnp.int32)
    keep = min(MAX_WINDOW, S)

    def mixer_in(h, i):
        u = rmsnorm(h, g_attn_norm[i]) @ w_in[i]
        return (u[..., :OFF_Z], u[..., OFF_Z:OFF_A], u[..., OFF_A:OFF_B], u[..., OFF_B:OFF_WIN], u[..., OFF_WIN:])

    def tail(h, oa, ob, p, i):
        h = h + jnp.concatenate([oa.astype(h.dtype), ob.astype(h.dtype)], axis=-1) @ w_out[i]
        N, L, _ = h.shape
        m = rmsnorm(h, g_ffn_norm[i]).reshape(N * L, D_MODEL)
        h = h + hier_moe(m, w_router_group[i], b_router_group[i], w_router_expert[i], b_router_expert[i],
                         w_exp_gate[i], w_exp_up[i], w_exp_down[i]).reshape(N, L, D_MODEL)
        return h + jax.nn.sigmoid(h @ w_ple_gate[i]) * (p.astype(h.dtype) @ w_ple_proj[i])

    hp, hs = x_prompt, x_sample
    wk_p, wv_p, cv_p, dl_p = [], [], [], []
    wk_s, wv_s, cv_s, dl_s = [], [], [], []
    for i in range(DEPTH):
        qkv_a, z_a, a_a, b_a, qkv_w = mixer_in(hp, i)
        oa, conv_new, s_new = delta_heads(qkv_a, z_a, a_a, b_a,
                                          jnp.zeros((NP, CONV_K - 1, 3 * A_WIDTH), hp.dtype),
                                          jnp.zeros((NP, HA, HEAD_DIM, HEAD_DIM), hp.dtype),
                                          w_conv[i], a_log[i], dt_bias[i], g_a_out[i])
        q, k, v = window_qkv(qkv_w, pos_p)
        ob = merge_dilations([dilated_band(q, k, v, w, d) for w, d in DILATIONS], g_b_out[i])
        hp = tail(hp, oa, ob, p_prompt[i], i)
        wk_p.append(k[:, S - keep:])
        wv_p.append(v[:, S - keep:])
        cv_p.append(conv_new)
        dl_p.append(s_new)

        qkv_a, z_a, a_a, b_a, qkv_w = mixer_in(hs, i)
        oa, conv_new, s_new = delta_heads(qkv_a, z_a, a_a, b_a, state_conv[i], state_delta[i],
                                          w_conv[i], a_log[i], dt_bias[i], g_a_out[i])
        q, k, v = window_qkv(qkv_w, pos_s)
        k_all = jnp.concatenate([cache_win_k[i].astype(k.dtype), k], axis=1)
        v_all = jnp.concatenate([cache_win_v[i].astype(v.dtype), v], axis=1)
        ob = merge_dilations(dilated_gather(q, k_all, v_all, n_past), g_b_out[i])
        hs = tail(hs, oa, ob, p_sample[i], i)
        wk_s.append(k)
        wv_s.append(v)
        cv_s.append(conv_new)
        dl_s.append(s_new)

    y_prompt = rmsnorm(hp, g_final)
    y_sample = rmsnorm(hs, g_final)
    return (y_prompt, y_sample, jnp.stack(wk_p), jnp.stack(wv_p), jnp.stack(cv_p), jnp.stack(dl_p),
            jnp.stack(wk_s), jnp.stack(wv_s), jnp.stack(cv_s), jnp.stack(dl_s))
```

```python
import math
import os
from contextlib import ExitStack

import numpy as np
import ml_dtypes
import concourse.bass as bass
import concourse.mybir as mybir
from concourse.bass_utils import run_bass_kernel_spmd

F32 = mybir.dt.float32
BF = mybir.dt.bfloat16
I32 = mybir.dt.int32
AF = mybir.ActivationFunctionType
ALU = mybir.AluOpType
AX = mybir.AxisListType

ENGS = ("pe", "act", "dve", "pool", "sp")

D = 1024
HD = 64
NH = 8
IN_W = 3600
OFF_Z = 1536
OFF_A = 2048
OFF_B = 2056
OFF_WIN = 2064
EPS = 1e-6
NWIN = 17


class Op:
    __slots__ = ("eng", "fn", "deps", "is_dma", "sig_idx", "idx", "dma_sem", "dma_val")

    def __init__(self, eng, fn, deps, is_dma):
        self.eng = eng
        self.fn = fn
        self.deps = deps
        self.is_dma = is_dma
        self.sig_idx = None
        self.dma_sem = None
        self.dma_val = None


class Rec:
    def __init__(self):
        self.ops = {e: [] for e in ENGS}
        self.last_w = {}
        self.readers = {}
        self.n_dma_sems = 16

    EXPAND = {"ps2": ("ps2a", "ps2b"), "po2": ("po2a", "po2b"), "ca": ("ca_l", "ca_h"), "cb": ("cb_l", "cb_h")}

    def op(self, eng, fn, reads=(), writes=(), dma=False):
        reads = [x for b in reads for x in self.EXPAND.get(b, (b,))]
        writes = [x for b in writes for x in self.EXPAND.get(b, (b,))]
        deps = []
        for b in reads:
            w = self.last_w.get(b)
            if w is not None:
                deps.append(w)
        for b in writes:
            w = self.last_w.get(b)
            if w is not None:
                deps.append(w)
            deps.extend(self.readers.get(b, {}).values())
        o = Op(eng, fn, deps, dma)
        o.idx = len(self.ops[eng])
        self.ops[eng].append(o)
        for b in reads:
            self.readers.setdefault(b, {})[eng if not dma else (eng, len(self.ops[eng]))] = o
        for b in writes:
            self.last_w[b] = o
            self.readers[b] = {}
        return o

    def setup(self, nc, es):
        self.esem = {e: es.enter_context(nc.semaphore("s_" + e)) for e in ENGS}
        self.dsem = [es.enter_context(nc.semaphore("d_%d" % i)) for i in range(self.n_dma_sems)]
        self.sig_base = {e: 0 for e in ENGS}
        self.dma_cnt = [0] * self.n_dma_sems
        self.dma_rr = {e: 0 for e in ENGS}
        self.prev_final = None

    def emit(self, nc):
        needs_sig = set()
        for e in ENGS:
            lastc = None
            for o in self.ops[e]:
                if not o.is_dma:
                    lastc = o
                for d in o.deps:
                    if d.is_dma or d is o:
                        continue
                    if d.eng != o.eng or d.eng != "pe":
                        needs_sig.add(id(d))
            if lastc is not None:
                needs_sig.add(id(lastc))
        for e in ENGS:
            c = self.sig_base[e]
            for o in self.ops[e]:
                if not o.is_dma and id(o) in needs_sig:
                    c += 1
                    o.sig_idx = c
            self.sig_base[e] = c
        half = self.n_dma_sems // 2
        for e in ENGS:
            base = half if e == "pool" else 0
            for o in self.ops[e]:
                if o.is_dma:
                    assert e in ("sp", "pool")
                    k = base + self.dma_rr[e] % half
                    self.dma_rr[e] += 1
                    self.dma_cnt[k] += 16
                    o.dma_sem = k
                    o.dma_val = self.dma_cnt[k]
        esem, dsem = self.esem, self.dsem
        ops = self.ops
        prev_final = self.prev_final
        with nc.Block() as block:

            def run(e, eng):
                waited = {}
                if prev_final is not None:
                    for key, val in prev_final.items():
                        if val > 0 and not (key[0] == "e" and key[1] == e):
                            s_ = dsem[key[1]] if key[0] == "d" else esem[key[1]]
                            eng.wait_ge(s_, val)
                        waited[key] = val
                for o in ops[e]:
                    w = {}
                    for d in o.deps:
                        if d is o:
                            continue
                        if d.is_dma:
                            key = ("d", d.dma_sem)
                            val = d.dma_val
                        else:
                            if d.eng == o.eng and d.eng == "pe":
                                continue
                            key = ("e", d.eng)
                            val = d.sig_idx
                        if val is None or waited.get(key, 0) >= val:
                            continue
                        if w.get(key, 0) < val:
                            w[key] = val
                    if o.is_dma and o.dma_val > 16:
                        key = ("d", o.dma_sem)
                        if waited.get(key, 0) < o.dma_val - 16 and w.get(key, 0) < o.dma_val - 16:
                            w[key] = o.dma_val - 16
                    for key, val in w.items():
                        waited[key] = val
                        s_ = dsem[key[1]] if key[0] == "d" else esem[key[1]]
                        eng.wait_ge(s_, val)
                    ins = o.fn(eng)
                    if o.is_dma:
                        ins.then_inc(dsem[o.dma_sem], 16)
                    elif o.sig_idx is not None:
                        ins.then_inc(esem[e], 1)
                fin = {}
                for o in ops[e]:
                    if o.is_dma:
                        fin[o.dma_sem] = max(fin.get(o.dma_sem, 0), o.dma_val)
                for k, v in fin.items():
                    eng.wait_ge(dsem[k], v)

            @block.tensor
            def _(eng):
                run("pe", eng)

            @block.scalar
            def _(eng):
                run("act", eng)

            @block.vector
            def _(eng):
                run("dve", eng)

            @block.gpsimd
            def _(eng):
                run("pool", eng)

            @block.sync
            def _(eng):
                run("sp", eng)
        fin = {("e", e): self.sig_base[e] for e in ENGS}
        for k in range(self.n_dma_sems):
            fin[("d", k)] = self.dma_cnt[k]
        self.prev_final = fin
        self.ops = {e: [] for e in ENGS}
        self.last_w = {}
        self.readers = {}


def build(NT, KEEP):
    NTT = NT + 1
    ROWS = NTT * 128
    nc = bass.Bass("TRN2", target_bir_lowering=False)
    R = Rec()
    esg = ExitStack()
    R.setup(nc, esg)
    es = ExitStack()

    def din(name, shape, dt=F32):
        return nc.dram_tensor(name, list(shape), dt, kind="ExternalInput").ap()

    def dout(name, shape, dt=F32):
        return nc.dram_tensor(name, list(shape), dt, kind="ExternalOutput").ap()

    def sb(name, shape, dt=F32):
        return es.enter_context(nc.sbuf_tensor(name, list(shape), dt))

    x_d = din("x", [ROWS, D])
    w_in_d = din("w_in", [D, IN_W])
    g_attn_d = din("g_attn", [1, D])
    cos_d = din("cos_t", [ROWS, 8])
    sin_d = din("sin_t", [ROWS, 8])
    ident_f_d = din("ident_f", [128, 128])
    ident_b_d = din("ident_b", [128, 128], BF)

    w_out_d = din("w_out", [D, D])
    g_ffn_d = din("g_ffn", [1, D])
    g_fin_d = din("g_fin", [1, D])
    w_rt_d = din("w_rt", [D, 36])
    b_rt_d = din("b_rt", [1, 36])
    weg_d = din("w_eg", [32, D, 512])
    weu_d = din("w_eu", [32, D, 512])
    wed_d = din("w_ed", [32, 512, D])
    wpg_d = din("w_pg", [D, D])
    wpp_d = din("w_pp", [256, D])
    p_d = din("p_all", [ROWS, 256])
    y_d = dout("y", [ROWS, D])
    wg_bf = nc.dram_tensor("wg_bf", [32 * 128, 4096], BF, kind="Internal").ap()
    wu_bf = nc.dram_tensor("wu_bf", [32 * 128, 4096], BF, kind="Internal").ap()
    wd_bf = nc.dram_tensor("wd_bf", [32 * 128, 4096], BF, kind="Internal").ap()
    sconv_d = din("state_conv", [16, 3, 1536])
    sdelta_d = din("state_delta", [16, 8, 64, 64])
    ck_d = din("cache_k", [16, 2048, 8, 64])
    cv_d = din("cache_v", [16, 2048, 8, 64])
    wconv_flat_d = din("wconv_flat", [1, 4 * 1536])
    alog128_d = din("alog128", [128, 1])
    dtb128_d = din("dtb128", [128, 1])
    cv_s_o = dout("cv_s", [16, 3, 1536])
    dl_s_o = dout("dl_s", [16, 8, 64, 64])
    pre_s_d = nc.dram_tensor("pre_s_scr", [16, 1536], F32, kind="Internal").ap()
    c3_d = nc.dram_tensor("c3_scr", [3, 16, 512], F32, kind="Internal").ap()
    z_s_d = nc.dram_tensor("z_s_scr", [16, 512], F32, kind="Internal").ap()
    qk_s_d = nc.dram_tensor("qk_s_scr", [2, 16, 512], F32, kind="Internal").ap()
    v_s_d = nc.dram_tensor("v_s_scr", [16, 512], F32, kind="Internal").ap()
    ab_s_d = nc.dram_tensor("ab_s_scr", [2, 16, 8], F32, kind="Internal").ap()
    oab_d = nc.dram_tensor("oab_scr", [2, 16, 512], F32, kind="Internal").ap()
    wk_o = dout("wk_p", [KEEP, 512])
    wv_o = dout("wv_p", [KEEP, 512])
    cv_o = dout("cv_p", [3, 1536])
    wk_s_o = dout("wk_s", [16, 512])
    wv_s_o = dout("wv_s", [16, 512])

    w_in = sb("w_in_sb", [128, 8, IN_W], BF)
    gB = sb("gB", [128, D])
    ident_f = sb("ident_f_sb", [128, 128])
    ident_b = sb("ident_b_sb", [128, 128], BF)
    xt = [sb("xt%d" % i, [128, D]) for i in range(2)]
    ss = sb("ss", [128, 1])
    rstd = sb("rstd", [128, 1])
    xn = sb("xn", [128, D], BF)
    xnT = sb("xnT", [128, 8, 128], BF)
    cs_t = [sb("cs_t%d" % i, [128, 16]) for i in range(2)]
    qk_sb = sb("qk_sb", [128, 16, 64])
    v_sb = sb("v_sb", [128, 512])

    pw = esg.enter_context(nc.psum_tensor("pw", [128, 1536], F32))
    pt = esg.enter_context(nc.psum_tensor("pt", [128, 512], F32))
    ps2 = esg.enter_context(nc.psum_tensor("ps2", [128, 1024], F32))
    po2 = esg.enter_context(nc.psum_tensor("po2", [128, 1024], F32))
    banks = [pw[:, 0:512], pw[:, 512:1024], pw[:, 1024:1536], pt[:, :], ps2[:, 0:512], ps2[:, 512:1024],
             po2[:, 0:512], po2[:, 512:1024]]
    kT_ring = sb("kT_ring", [128, 4, NWIN * 128], BF)
    V_ring = sb("V_ring", [128, NWIN, 8, 65], BF)
    qTe = sb("qTe", [128, 4, 128], BF)
    qTo = sb("qTo", [128, 4, 128], BF)
    qkb = sb("qkb", [128, 1024], BF)
    pexp = [sb("pexp%d" % i, [128, 8, 128], BF) for i in range(2)]
    pm = [sb("pm%d" % i, [128, 8, 128], BF) for i in range(2)]
    Cm = sb("Cm", [128, NWIN, 128], BF)
    rden = sb("rden", [128, 8])
    ssb = sb("ssb", [128, 8])
    gbB = sb("gbB", [128, 64])
    o_cat = sb("o_cat", [128, 1024], BF)
    wconv_d = din("wconvT", [128, 48])
    alog_d = din("a_log", [1, 8])
    dtb_d = din("dt_bias", [1, 8])
    g_a_d = din("g_a", [1, 64])
    tri_d = din("triF", [128, 128])
    ones_d = din("onesF", [128, 128])
    tris_d = din("triS", [128, 128])
    pidx_d = din("pidx", [128, 1])
    b128_d = din("b128", [1, (2 * ROWS) // 256 + 32])
    trin_d = din("triN", [128, 128])
    mUi_d = din("maskUi", [128, 128])
    mUs_d = din("maskUs", [128, 128])
    mLs_d = din("maskLs", [128, 128])
    dl_o = dout("dl_p", [128, 256])
    ocat_d = nc.dram_tensor("ocat_scr", [ROWS, 1024], BF, kind="Internal").ap()
    wT = sb("wT", [128, 12, 4])
    nexpA = sb("nexpA", [128, 8])
    dtbB = sb("dtbB", [128, 8])
    gaB = sb("gaB", [128, 64])
    triF = sb("triF_sb", [128, 128])
    onesF = sb("onesF_sb", [128, 128])
    mUi = sb("mUi", [128, 128])
    mUs = sb("mUs", [128, 128])
    mLs = sb("mLs", [128, 128])
    xc = sb("xc", [128, 12, 131])
    ca = sb("ca", [128, 1536])
    cb = sb("cb", [128, 1536])
    pre_sb = ca
    sq = cb[:, 0:1024]
    rn = sb("rn", [128, 16])
    zs = sb("zs", [128, 512])
    sm = sb("sm", [128, 80])
    kp_f = sb("kp_f", [128, 8, 64])
    kpqs_b = sb("kpqs_b", [128, 1024], BF)
    kgqg_b = sb("kgqg_b", [128, 1024], BF)
    kd_b = sb("kd_b", [128, 512], BF)
    vb_f = sb("vb_f", [128, 512])
    r_f = vb_f
    kpT = sb("kpT", [128, 4, 128], BF)
    kpTe = sb("kpTe", [128, 4, 128], BF)
    kpTo = sb("kpTo", [128, 4, 128], BF)
    qsTe = sb("qsTe", [128, 4, 128], BF)
    qsTo = sb("qsTo", [128, 4, 128], BF)
    kgqgT = sb("kgqgT", [128, 8, 128], BF)
    LaB = sb("LaB", [128, 8, 128])
    dtmp = LaB
    triN = sb("triN_sb", [128, 128])
    decUi = sb("decUi", [128, 8, 128], BF)
    Am = [sb("Am%d" % i, [128, 8, 128]) for i in range(2)]
    Bm = [sb("Bm%d" % i, [128, 8, 128]) for i in range(2)]
    Rm = sb("Rm", [128, 8, 128])
    MTs = sb("MTs", [128, 8, 128], BF)
    S_sb = sb("S_sb", [128, 4, 64])
    S_bd = sb("S_bd", [128, 4, 128], BF)
    x_b = sb("x_b", [128, 512], BF)
    o_f = sb("o_f", [128, 8, 64])
    ob_f = o_f
    rt = [o_f[:, 2 * i:2 * i + 2, :].rearrange("p a (h d) -> p (a h) d", d=8) for i in range(4)]
    cm_d = din("cmask", [128, NWIN * 128], BF)
    g_b_d = din("g_b", [1, 64])

    def dma(out, in_, reads=(), writes=(), eng="sp"):
        return R.op(eng, lambda e: e.dma_start(out=out, in_=in_), reads, writes, dma=True)

    def act(out, in_, func, reads=(), writes=(), **kw):
        return R.op("act", lambda e: e.activation(out=out, in_=in_, func=func, **kw), reads, writes)

    def mm(out, lhsT, rhs, start, stop, reads=(), writes=(), sgc=False):
        return R.op("pe", lambda e: e.matmul(out, lhsT, rhs, start=start, stop=stop, skip_group_check=sgc),
                    reads, writes)

    def tr(out, in_, ident, reads=(), writes=()):
        return R.op("pe", lambda e: e.transpose(out, in_, ident), reads, writes)

    def tt(eng, out, in0, in1, op, reads=(), writes=()):
        return R.op(eng, lambda e: e.tensor_tensor(out, in0, in1, op), reads, writes)

    def ts(eng, out, in0, s1, s2, op0, op1=None, reads=(), writes=()):
        if op1 is None:
            return R.op(eng, lambda e: e.tensor_scalar(out, in0, s1, None, op0), reads, writes)
        return R.op(eng, lambda e: e.tensor_scalar(out, in0, s1, s2, op0, op1), reads, writes)

    def stt(out, in0, scalar, in1, op0, op1, reads=(), writes=()):
        return R.op("dve", lambda e: e.scalar_tensor_tensor(out, in0, scalar, in1, op0, op1), reads, writes)

    def cp(eng, out, in_, reads=(), writes=()):
        if eng == "act":
            return R.op("act", lambda e: e.copy(out, in_), reads, writes)
        return R.op(eng, lambda e: e.tensor_copy(out, in_), reads, writes)

    def red(out, in_, op, reads=(), writes=()):
        return R.op("dve", lambda e: e.tensor_reduce(out, in_, AX.X, op), reads, writes)

    def recip(out, in_, reads=(), writes=()):
        return R.op("dve", lambda e: e.reciprocal(out, in_), reads, writes)

    dma(ident_f[:], ident_f_d, writes=["ident_f"])
    dma(ident_b[:], ident_b_d, writes=["ident_b"])
    dma(gB[:], g_attn_d.partition_broadcast(128), writes=["gB"])
    w_in_v = w_in_d.rearrange("(k p) n -> p k n", p=128)
    for k in range(8):
        dma(w_in[:, k, :], w_in_v[:, k, :], writes=["w_in"], eng="pool")

    for e_ in range(32 if not os.environ.get('NOCONV') else 0):
        dma(wg_bf[e_ * 128:(e_ + 1) * 128, :].rearrange("p (k n) -> p k n", k=8),
            weg_d[e_].rearrange("(k p) n -> p k n", p=128), eng="pool")
        dma(wu_bf[e_ * 128:(e_ + 1) * 128, :].rearrange("p (k n) -> p k n", k=8),
            weu_d[e_].rearrange("(k p) n -> p k n", p=128), eng="pool")
        dma(wd_bf[e_ * 128:(e_ + 1) * 128, :].rearrange("p (c n) -> p c n", c=4),
            wed_d[e_].rearrange("(c p) n -> p c n", p=128), eng="pool")

    def rmsnorm_T(t):
        s = t % 2
        dma(xt[s][:], x_d[t * 128:(t + 1) * 128, :], writes=["xt%d" % s])
        dma(cs_t[s][:, 0:8], cos_d[t * 128:(t + 1) * 128, :], writes=["cs%d" % s])
        dma(cs_t[s][:, 8:16], sin_d[t * 128:(t + 1) * 128, :], writes=["cs%d" % s])
        act(sq[:], xt[s][:], AF.Square, reads=["xt%d" % s], writes=["cb"])
        red(ss[:], sq[:], ALU.add, reads=["cb"], writes=["ss"])
        ts("dve", ss[:], ss[:], 1.0 / D, EPS, ALU.mult, ALU.add, reads=["ss"], writes=["ss"])
        act(ss[:], ss[:], AF.Sqrt, reads=["ss"], writes=["ss"])
        recip(rstd[:], ss[:], reads=["ss"], writes=["rstd"])
        stt(xn[:], xt[s][:], rstd[:], gB[:], ALU.mult, ALU.mult,
            reads=["xt%d" % s, "rstd", "gB"], writes=["xn"])
        pT = banks[3][:].bitcast(BF)
        for k in range(8):
            tr(pT[:, k * 128:(k + 1) * 128], xn[:, k * 128:(k + 1) * 128], ident_b[:],
               reads=["xn", "ident_b"], writes=["b3"])
        cp("act", xnT[:].rearrange("p k t -> p (k t)"), pT, reads=["b3"], writes=["xnT"])

    def inproj_tm(col0, ncols, bank_ids, bname):
        done = 0
        bi = 0
        while done < ncols:
            n = min(512, ncols - done)
            for k in range(8):
                mm(banks[bank_ids[bi]][:, 0:n], xnT[:, k, :], w_in[:, k, col0 + done:col0 + done + n],
                   start=(k == 0), stop=(k == 7), reads=["xnT", "w_in"], writes=[bname[bi]])
            done += n
            bi += 1

    def window_qkv(t):
        s = t % 2
        inproj_tm(OFF_WIN, 1536, [0, 1, 2], ["b0", "b1", "b2"])
        for j, b in enumerate((0, 1)):
            pv = banks[b][:].rearrange("p (h d) -> p h d", d=64)
            qs = qk_sb[:, j * 8:(j + 1) * 8, :]
            cosB = cs_t[s][:, None, 0:8].to_broadcast([128, 8, 8])
            sinB = cs_t[s][:, None, 8:16].to_broadcast([128, 8, 8])
            x1 = pv[:, :, 0:8]
            x2 = pv[:, :, 8:16]
            r0 = rt[0][:, j * 8:(j + 1) * 8, :]
            r1 = rt[1][:, j * 8:(j + 1) * 8, :]
            r2 = rt[2][:, j * 8:(j + 1) * 8, :]
            r3 = rt[3][:, j * 8:(j + 1) * 8, :]
            bn = "b%d" % b
            tt("dve", r0, x1, cosB, ALU.mult, reads=[bn, "cs%d" % s], writes=["o_f"])
            tt("dve", r1, x2, sinB, ALU.mult, reads=[bn, "cs%d" % s], writes=["o_f"])
            tt("dve", r2, x2, cosB, ALU.mult, reads=[bn, "cs%d" % s], writes=["o_f"])
            tt("dve", r3, x1, sinB, ALU.mult, reads=[bn, "cs%d" % s], writes=["o_f"])
            tt("dve", qs[:, :, 0:8], r0, r1, ALU.subtract, reads=["o_f"], writes=["qk_sb"])
            tt("dve", qs[:, :, 8:16], r2, r3, ALU.add, reads=["o_f"], writes=["qk_sb"])
            cp("act", qs[:, :, 16:64], pv[:, :, 16:64], reads=[bn], writes=["qk_sb"])
        cp("act", v_sb[:], banks[2][:], reads=["b2"], writes=["v_sb"])

    def outputs_kv(t):
        if t < NT:
            first = NT - KEEP // 128
            if t >= first:
                r0 = (t - first) * 128
                dma(wk_o[r0:r0 + 128, :], qk_sb[:, 8:16, :].rearrange("p h d -> p (h d)"), reads=["qk_sb"])
                dma(wv_o[r0:r0 + 128, :], v_sb[:], reads=["v_sb"])
        else:
            dma(wk_s_o[:, :], qk_sb[0:16, 8:16, :].rearrange("p h d -> p (h d)"), reads=["qk_sb"])
            dma(wv_s_o[:, :], v_sb[0:16, :], reads=["v_sb"])

    def preconv_tm(t):
        inproj_tm(0, 1536, [0, 1, 2], ["b0", "b1", "b2"])
        for j, b in enumerate((0, 1, 2)):
            cp("act", pre_sb[:, j * 512:(j + 1) * 512], banks[b][:], reads=["b%d" % b], writes=["ca"])
        if t == NT - 1:
            dma(cv_o[:, :], pre_sb[125:128, :], reads=["ca"])
        if t == NT:
            dma(pre_s_d[:, :], pre_sb[0:16, :], reads=["ca"])
            for k in range(8):
                mm(ps2[:, 0:512], xnT[:, k, :], w_in[:, k, OFF_Z:OFF_Z + 512], k == 0, k == 7, reads=["xnT", "w_in"],
                   writes=["ps2"])
            for k in range(8):
                mm(ps2[:, 512:528], xnT[:, k, :], w_in[:, k, OFF_A:OFF_A + 16], k == 0, k == 7, reads=["xnT", "w_in"],
                   writes=["ps2"])
            cp("act", zs[:], ps2[:, 0:512], reads=["ps2"], writes=["zs"])
            cp("act", sm[:, 0:16], ps2[:, 512:528], reads=["ps2"], writes=["sm_xa"])
            dma(z_s_d[:, :], zs[0:16, :], reads=["zs"])
            dma(ab_s_d[0], sm[0:16, 0:8], reads=["sm_xa"])
            dma(ab_s_d[1], sm[0:16, 8:16], reads=["sm_xa"])
            dma(qk_s_d[0], qk_sb[0:16, 0:8, :].rearrange("p h d -> p (h d)"), reads=["qk_sb"])
            dma(qk_s_d[1], qk_sb[0:16, 8:16, :].rearrange("p h d -> p (h d)"), reads=["qk_sb"])
            dma(v_s_d[:, :], v_sb[0:16, :], reads=["v_sb"])

    dma(Cm[:].rearrange("p a b -> p (a b)"), cm_d, writes=["Cm"])
    dma(gbB[:], g_b_d.partition_broadcast(128), writes=["gbB"])
    R.op("pool", lambda e: e.memset(V_ring[:].rearrange("p a h d -> p (a h d)"), 1.0), writes=["V_ring"])
    R.op("pool", lambda e: e.memset(qTe[:].rearrange("p a t -> p (a t)"), 0.0), writes=["qT"])
    R.op("pool", lambda e: e.memset(qTo[:].rearrange("p a t -> p (a t)"), 0.0), writes=["qT"])

    def attn_finish(np_, tag):
        pov = po2[0:np_, :].rearrange("p (h d) -> p h d", d=128)
        R.op("dve", lambda e: e.reciprocal(rden[0:np_, :], pov[:, :, 64]), ["po2"], ["rden"])
        tt("dve", ob_f[0:np_], pov[:, :, 0:64], rden[0:np_, :, None].to_broadcast([np_, 8, 64]), ALU.mult,
           reads=["po2", "rden"], writes=["o_f"])
        act(sq[0:np_, 0:512], ob_f[0:np_].rearrange("p h d -> p (h d)"), AF.Square, reads=["o_f"], writes=["cb"])
        red(ssb[0:np_, :], sq[0:np_, 0:512].rearrange("p (h d) -> p h d", d=64), ALU.add, reads=["cb"], writes=["ssb"])
        ts("dve", ssb[0:np_, :], ssb[0:np_, :], 1.0 / 64, EPS, ALU.mult, ALU.add, reads=["ssb"], writes=["ssb"])
        act(ssb[0:np_, :], ssb[0:np_, :], AF.Sqrt, reads=["ssb"], writes=["ssb"])
        recip(ssb[0:np_, :], ssb[0:np_, :], reads=["ssb"], writes=["ssb"])
        tt("dve", ob_f[0:np_], ob_f[0:np_], ssb[0:np_, :, None].to_broadcast([np_, 8, 64]), ALU.mult,
           reads=["o_f", "ssb"], writes=["o_f"])
        tt("dve", o_cat[0:np_, 512:1024].rearrange("p (h d) -> p h d", d=64), ob_f[0:np_],
           gbB[0:np_, None, :].to_broadcast([np_, 8, 64]), ALU.mult, reads=["o_f", "gbB"], writes=["o_cat"])

    def attn_prompt(t):
        sl = t % NWIN
        cp("act", qkb[:], qk_sb[:].rearrange("p h d -> p (h d)"), reads=["qk_sb"], writes=["qkb"])
        cp("pool", V_ring[:, sl, :, 0:64], v_sb[:].rearrange("p (h d) -> p h d", d=64),
           reads=["v_sb"], writes=["V_ring"])
        pT = pt[:, :].bitcast(BF)
        for c in range(8):
            tr(pT[:, c * 128:(c + 1) * 128], qkb[:, c * 128:(c + 1) * 128], ident_b[:],
               reads=["qkb", "ident_b"], writes=["b3"])
        cp("act", qTe[0:64].rearrange("p a t -> p (a t)"), pT[0:64, 0:512], reads=["b3"], writes=["qT"])
        cp("act", qTo[64:128].rearrange("p a t -> p (a t)"), pT[64:128, 0:512], reads=["b3"], writes=["qT"])
        cp("act", kT_ring[:, :, sl * 128:(sl + 1) * 128], pT[:, 512:1024].rearrange("p (a t) -> p a t", t=128),
           reads=["b3"], writes=["kT_ring"])
        LV = int(os.environ.get('ATT_LEVEL', '9'))
        nd = min(t, 16) + 1
        nst = 2 * nd

        def scores(st):
            dl, hg = st // 2, st % 2
            s2 = (t - dl) % NWIN
            buf = ps2[:, (st % 2) * 512:(st % 2 + 1) * 512]
            bn = ["ps2a" if st % 2 == 0 else "ps2b"]
            for hh in range(4):
                h = 4 * hg + hh
                hp, pr = h % 2, h // 2
                mm(buf[:, hh * 128:(hh + 1) * 128], kT_ring[:, pr, s2 * 128:(s2 + 1) * 128],
                   (qTo if hp else qTe)[:, pr, :], True, True, reads=["kT_ring", "qT"], writes=bn)
            pe_ = pexp[st % 2][:, 0:4, :]
            act(pe_.rearrange("p h t -> p (h t)"), buf, AF.Exp, reads=bn, writes=["pexp%d" % (st % 2)], scale=0.125)
            tt("pool" if st % 2 == 0 else "dve", pm[st % 2][:, 0:4, :], pe_,
               Cm[:, dl:dl + 1, :].to_broadcast([128, 4, 128]), ALU.mult,
               reads=["pexp%d" % (st % 2), "Cm"], writes=["pm%d" % (st % 2)])

        def pv(st):
            dl, hg = st // 2, st % 2
            s2 = (t - dl) % NWIN
            pmb = pm[st % 2]
            for hh in range(4):
                h = 4 * hg + hh
                mm(po2[:, h * 128:h * 128 + 65], pmb[:, hh, :], V_ring[:, s2, h, :], dl == 0 and hh == 0, dl == nd - 1,
                   reads=["pm%d" % (st % 2), "V_ring"], writes=["po2a" if hg == 0 else "po2b"], sgc=True)

        def gen():
            scores(0)
            yield
            for st in range(nst):
                if st + 1 < nst:
                    scores(st + 1)
                pv(st)
                yield
        return gen()

    def attn_done(t):
        LV = 9
        if LV < 5:
            return
        attn_finish(128, "p")


    for nm, tl, dd in (("triN", triN, trin_d), ("triF", triF, tri_d), ("onesF", onesF, ones_d), ("mUi", mUi, mUi_d), ("mUs", mUs, mUs_d),
                       ("mLs", mLs, mLs_d)):
        dma(tl[:], dd, writes=[nm])
    dma(wT[:].rearrange("p c j -> p (c j)"), wconv_d, writes=["wT"])
    dma(nexpA[:], alog_d.partition_broadcast(128), writes=["nexpA"])
    dma(dtbB[:], dtb_d.partition_broadcast(128), writes=["dtbB"])
    dma(gaB[:], g_a_d.partition_broadcast(128), writes=["gaB"])
    act(nexpA[:], nexpA[:], AF.Exp, reads=["nexpA"], writes=["nexpA"])
    ts("dve", nexpA[:], nexpA[:], -1.0, None, ALU.mult, reads=["nexpA"], writes=["nexpA"])
    R.op("pool", lambda e: e.memset(xc[:].rearrange("p c t -> p (c t)"), 0.0), writes=["xc"])
    R.op("pool", lambda e: e.memset(S_sb[:].rearrange("p a d -> p (a d)"), 0.0), writes=["S_sb"])
    R.op("pool", lambda e: e.memset(S_bd[:].rearrange("p a d -> p (a d)"), 0.0), writes=["S_bd"])
    for tl, nm in ((kpTe, "kpTe"), (kpTo, "kpTo"), (qsTe, "qsTe"), (qsTo, "qsTo")):
        R.op("pool", (lambda tl: (lambda e: e.memset(tl[:].rearrange("p a t -> p (a t)"), 0.0)))(tl), writes=[nm])

    def bc8(ap, n=64):
        return ap[:, :, None].to_broadcast([128, 8, n])

    GLV = int(os.environ.get('GDN_LEVEL', '9'))
    G4 = int(os.environ.get('G4', '99'))
    G7 = int(os.environ.get('G7', '99'))

    def gdn_front(t):
        for c in range(12):
            for k in range(8):
                mm(pw[:, c * 128:(c + 1) * 128], w_in[:, k, c * 128:(c + 1) * 128], xnT[:, k, :], k == 0, k == 7,
                   reads=["xnT", "w_in"], writes=["b0", "b1", "b2"])
        if t > 0:
            cp("pool", xc[:, :, 0:3], xc[:, :, 128:131], reads=["xc"], writes=["xc"])
        cp("act", xc[:, :, 3:131], pw[:, :].rearrange("p (c t) -> p c t", t=128), reads=["b0", "b1", "b2"], writes=["xc"])
        ca3 = ca[:].rearrange("p (c t) -> p c t", t=128)
        cb3 = cb[:].rearrange("p (c t) -> p c t", t=128)

        def wb(j):
            return wT[:, :, j:j + 1].to_broadcast([128, 12, 128])
        for eng_, lo_, hi_, sfx in (("pool", 0, 5, "_l"), ("dve", 5, 12, "_h")):
            cah, cbh = ca3[:, lo_:hi_, :], cb3[:, lo_:hi_, :]

            def wbh(j, lo_=lo_, hi_=hi_):
                return wT[:, lo_:hi_, j:j + 1].to_broadcast([128, hi_ - lo_, 128])
            tt(eng_, cah, xc[:, lo_:hi_, 3:131], wbh(3), ALU.mult, reads=["xc", "wT"], writes=["ca" + sfx])
            for j in (2, 1, 0):
                tt(eng_, cbh, xc[:, lo_:hi_, j:j + 128], wbh(j), ALU.mult, reads=["xc", "wT"], writes=["cb" + sfx])
                tt(eng_, cah, cah, cbh, ALU.add, reads=["ca" + sfx, "cb" + sfx], writes=["ca" + sfx])
        act(cb[:], ca[:], AF.Silu, reads=["ca"], writes=["cb"])

    def gdn_prompt(t):
        ca3 = ca[:].rearrange("p (c t) -> p c t", t=128)
        if GLV <= 1:
            return
        for c in range(12):
            tr(pw[:, c * 128:(c + 1) * 128], cb[:, c * 128:(c + 1) * 128], ident_f[:], reads=["cb", "ident_f"],
               writes=["b0", "b1", "b2"])
        cp("act", ca[:], pw[:, :], reads=["b0", "b1", "b2"], writes=["ca"])
        ca_qk = ca[:, 0:1024].rearrange("p (h d) -> p h d", d=64)
        ca_q = ca[:, 0:512].rearrange("p (h d) -> p h d", d=64)
        ca_k = ca[:, 512:1024].rearrange("p (h d) -> p h d", d=64)
        ca_v = ca[:, 1024:1536].rearrange("p (h d) -> p h d", d=64)
        act(sq[:], ca[:, 0:1024], AF.Square, reads=["ca"], writes=["cb"])
        red(rn[:], sq[:].rearrange("p (h d) -> p h d", d=64), ALU.add, reads=["cb"], writes=["rn"])
        ts("dve", rn[:], rn[:], EPS, None, ALU.add, reads=["rn"], writes=["rn"])
        act(rn[:], rn[:], AF.Sqrt, reads=["rn"], writes=["rn"])
        recip(rn[:], rn[:], reads=["rn"], writes=["rn"])
        tt("dve", ca_qk, ca_qk, rn[:, :, None].to_broadcast([128, 16, 64]), ALU.mult, reads=["ca", "rn"], writes=["ca"])
        if GLV <= 2:
            return
        for k in range(8):
            mm(ps2[:, 0:512], xnT[:, k, :], w_in[:, k, OFF_Z:OFF_Z + 512], k == 0, k == 7, reads=["xnT", "w_in"],
               writes=["ps2"])
        for k in range(8):
            mm(ps2[:, 512:528], xnT[:, k, :], w_in[:, k, OFF_A:OFF_A + 16], k == 0, k == 7, reads=["xnT", "w_in"],
               writes=["ps2"])
        act(zs[:], ps2[:, 0:512], AF.Silu, reads=["ps2"], writes=["zs"])
        xa, la, beta, sbt = sm[:, 0:8], sm[:, 8:16], sm[:, 16:24], sm[:, 24:32]
        g_, eg, egl, eglg, dgl, sso = sm[:, 32:40], sm[:, 40:48], sm[:, 48:56], sm[:, 56:64], sm[:, 64:72], sm[:, 72:80]
        tt("dve", xa, ps2[:, 512:520], dtbB[:], ALU.add, reads=["ps2", "dtbB"], writes=["sm_xa"])
        act(beta, ps2[:, 520:528], AF.Sigmoid, reads=["ps2"], writes=["sm_beta"])
        act(sbt, beta, AF.Sqrt, reads=["sm_beta"], writes=["sm_sbt"])
        act(xa, xa, AF.Exp, reads=["sm_xa"], writes=["sm_xa"])
        act(xa, xa, AF.Ln, reads=["sm_xa"], writes=["sm_xa"], bias=1.0)
        tt("dve", la, xa, nexpA[:], ALU.mult, reads=["sm_xa", "nexpA"], writes=["sm_la"])
        mm(pt[:, 0:8], triF[:], la, True, True, reads=["triF", "sm_la"], writes=["b3"])
        mm(pt[:, 8:16], onesF[:], la, True, True, reads=["onesF", "sm_la"], writes=["b3"])
        cp("dve", g_, pt[:, 0:8], reads=["b3"], writes=["sm_g"])
        act(eg, pt[:, 0:8], AF.Exp, reads=["b3"], writes=["sm_eg"])
        act(egl, pt[:, 8:16], AF.Exp, reads=["b3"], writes=["sm_egl"])
        tt("dve", dgl, pt[:, 8:16], g_, ALU.subtract, reads=["b3", "sm_g"], writes=["sm_dgl"])
        act(eglg, dgl, AF.Exp, reads=["sm_dgl"], writes=["sm_eglg"])
        if GLV <= 3:
            return
        if G4 < 1:
            return
        tt("dve", kp_f[:], ca_k, bc8(sbt), ALU.mult, reads=["ca", "sm_sbt"], writes=["kp_f"])
        if G4 < 2:
            return
        cp("act", kpqs_b[:, 0:512], kp_f[:].rearrange("p h d -> p (h d)"), reads=["kp_f"], writes=["kpqs_b"])
        if G4 < 3:
            return
        act(kpqs_b[:, 512:1024], ca[:, 0:512], AF.Copy, reads=["ca"], writes=["kpqs_b"], scale=0.125)
        if G4 < 4:
            return
        tt("pool", kgqg_b[:, 0:512].rearrange("p (h d) -> p h d", d=64), kp_f[:], bc8(eg), ALU.mult,
           reads=["kp_f", "sm_eg"], writes=["kgqg_b"])
        if G4 < 5:
            return
        stt(kgqg_b[:, 512:1024].rearrange("p (h d) -> p h d", d=64), ca_q, 0.125, bc8(eg), ALU.mult, ALU.mult,
            reads=["ca", "sm_eg"], writes=["kgqg_b"])
        if G4 < 6:
            return
        tt("pool", kd_b[:].rearrange("p (h d) -> p h d", d=64), kp_f[:], bc8(eglg), ALU.mult,
           reads=["kp_f", "sm_eglg"], writes=["kd_b"])
        if G4 < 7:
            return
        tt("dve", vb_f[:].rearrange("p (h d) -> p h d", d=64), ca_v, bc8(sbt), ALU.mult, reads=["ca", "sm_sbt"],
           writes=["vb_f"])
        if G4 < 8:
            return
        ps2b = ps2[:, :].bitcast(BF)
        if G4 < 9:
            return
        for i in range(8):
            tr(ps2b[:, i * 128:(i + 1) * 128], kpqs_b[:, i * 128:(i + 1) * 128], ident_b[:],
               reads=["kpqs_b", "ident_b"], writes=["ps2"])
        if G4 < 10:
            return
        for i in range(8):
            tr(ps2b[:, 1024 + i * 128:1024 + (i + 1) * 128], kgqg_b[:, i * 128:(i + 1) * 128], ident_b[:],
               reads=["kgqg_b", "ident_b"], writes=["ps2"])
        if G4 < 11:
            return
        cp("act", kpT[:].rearrange("p a t -> p (a t)"), ps2b[:, 0:512], reads=["ps2"], writes=["kpT"])
        if G4 < 12:
            return
        cp("act", kpTe[0:64].rearrange("p a t -> p (a t)"), ps2b[0:64, 0:512], reads=["ps2"], writes=["kpTe"])
        if G4 < 13:
            return
        cp("act", kpTo[64:128].rearrange("p a t -> p (a t)"), ps2b[64:128, 0:512], reads=["ps2"], writes=["kpTo"])
        if G4 < 14:
            return
        cp("act", qsTe[0:64].rearrange("p a t -> p (a t)"), ps2b[0:64, 512:1024], reads=["ps2"], writes=["qsTe"])
        if G4 < 15:
            return
        cp("act", qsTo[64:128].rearrange("p a t -> p (a t)"), ps2b[64:128, 512:1024], reads=["ps2"], writes=["qsTo"])
        if G4 < 16:
            return
        cp("act", kgqgT[:].rearrange("p a t -> p (a t)"), ps2b[:, 1024:2048], reads=["ps2"], writes=["kgqgT"])
        if GLV <= 4:
            return
        cp("dve", LaB[:], la[:, :, None].to_broadcast([128, 8, 128]), reads=["sm_la"], writes=["LaB"])
        for h in range(8):
            mm(po2[:, h * 128:(h + 1) * 128], LaB[:, h, :], triF[:], True, False, reads=["LaB", "triF"], writes=["po2"])
            mm(po2[:, h * 128:(h + 1) * 128], triN[:], LaB[:, h, :], False, True, reads=["LaB", "triN"],
               writes=["po2"])
        po2v = po2[:, :].rearrange("p (h t) -> p h t", t=128)
        ps2v = ps2[:, :].rearrange("p (h t) -> p h t", t=128)

        def mb(m):
            return m[:, None, :].to_broadcast([128, 8, 128])
        for h in range(8):
            hp, pr = h % 2, h // 2
            mm(ps2[:, h * 128:(h + 1) * 128], kpT[:, pr, :], (kpTo if hp else kpTe)[:, pr, :], True, True,
               reads=["kpT", "kpTe", "kpTo"], writes=["ps2"])
        stt(dtmp[:], po2v, -1.0, mb(mLs), ALU.mult, ALU.add, reads=["po2", "mLs"], writes=["LaB"])
        act(dtmp[:], dtmp[:], AF.Exp, reads=["LaB"], writes=["LaB"])
        tt("dve", Am[0][:], ps2v, dtmp[:], ALU.mult, reads=["ps2", "LaB"], writes=["Am0_0", "Am0_1"])
        tt("dve", dtmp[:], po2v, mb(mUs), ALU.add, reads=["po2", "mUs"], writes=["LaB"])
        act(dtmp[:], dtmp[:], AF.Exp, reads=["LaB"], writes=["LaB"])
        tt("dve", Bm[0][:], ps2v, dtmp[:], ALU.mult, reads=["ps2", "LaB"], writes=["Bm0_0", "Bm0_1"])
        tt("dve", dtmp[:], po2v, mb(mUi), ALU.add, reads=["po2", "mUi"], writes=["LaB"])
        act(decUi[:], dtmp[:], AF.Exp, reads=["LaB"], writes=["decUi"])
        for h in range(8):
            hp, pr = h % 2, h // 2
            mm(ps2[:, h * 128:(h + 1) * 128], kpT[:, pr, :], (qsTo if hp else qsTe)[:, pr, :], True, True,
               reads=["kpT", "qsTe", "qsTo"], writes=["ps2"])
        tt("dve", MTs[:], ps2v, decUi[:], ALU.mult, reads=["ps2", "decUi"], writes=["MTs"])
        if GLV <= 5:
            return
        tt("pool", Rm[:], ident_f[:, None, :].to_broadcast([128, 8, 128]), Bm[0][:], ALU.subtract,
           reads=["ident_f", "Bm0_0", "Bm0_1"], writes=["Rm0", "Rm1"])
        def neu():
            cur = 0
            for lvl in range(6):
                last = lvl == 5
                nx = 1 - cur
                for half in range(2):
                    hs = range(4 * half, 4 * half + 4)
                    for h in hs:
                        mm(pw[:, (h % 4) * 128:(h % 4 + 1) * 128], Bm[cur][:, h, :], Am[cur][:, h, :], True, True,
                           reads=["Am%d_%d" % (cur, half), "Bm%d_%d" % (cur, half)], writes=["b0"])
                    if not last:
                        for h in hs:
                            mm(pw[:, 512 + (h % 4) * 128:512 + (h % 4 + 1) * 128], Am[cur][:, h, :], Bm[cur][:, h, :],
                               True, True, reads=["Am%d_%d" % (cur, half), "Bm%d_%d" % (cur, half)], writes=["b1"])
                    cp("act", Am[nx][:, 4 * half:4 * half + 4, :].rearrange("p h t -> p (h t)"), pw[:, 0:512],
                       reads=["b0"], writes=["Am%d_%d" % (nx, half)])
                    if not last:
                        ts("dve", Bm[nx][:, 4 * half:4 * half + 4, :].rearrange("p h t -> p (h t)"), pw[:, 512:1024],
                           1.0, None, ALU.mult, reads=["b1"], writes=["Bm%d_%d" % (nx, half)])
                    yield
                    for h in hs:
                        mm(pw[:, 1024 + (h % 4) * 128:1024 + (h % 4 + 1) * 128], Am[nx][:, h, :], Rm[:, h, :], True,
                           True, reads=["Am%d_%d" % (nx, half), "Rm%d" % half], writes=["b2"])
                    rv = Rm[:, 4 * half:4 * half + 4, :].rearrange("p h t -> p (h t)")
                    tt("dve", rv, rv, pw[:, 1024:1536], ALU.add, reads=["Rm%d" % half, "b2"], writes=["Rm%d" % half])
                    yield
                cur = nx
        return neu()

    def gdn_rec(t):
        xa, la, beta, sbt = sm[:, 0:8], sm[:, 8:16], sm[:, 16:24], sm[:, 24:32]
        g_, eg, egl, eglg, dgl, sso = sm[:, 32:40], sm[:, 40:48], sm[:, 48:56], sm[:, 56:64], sm[:, 64:72], sm[:, 72:80]
        if GLV <= 6:
            return
        kgT = kgqgT[:, 0:4, :]
        qgT = kgqgT[:, 4:8, :]
        if G7 < 1:
            return
        for pr in range(4):
            mm(pt[:, pr * 128:(pr + 1) * 128], kgT[:, pr, :], S_bd[:, pr, :], True, True, reads=["kgqgT", "S_bd"],
               writes=["b3"])
        if G7 < 2:
            return
        tt("dve", r_f[:], vb_f[:], pt[:, :], ALU.subtract, reads=["vb_f", "b3"], writes=["vb_f"])
        if G7 < 3:
            return
        for h in range(8):
            mm(pt[:, h * 64:(h + 1) * 64], Rm[:, h, :], r_f[:, h * 64:(h + 1) * 64], True, True, reads=["Rm0", "Rm1", "vb_f"],
               writes=["b3"])
        if G7 < 4:
            return
        cp("act", x_b[:], pt[:, :], reads=["b3"], writes=["x_b"])
        if G7 < 5:
            return
        for pr in range(4):
            mm(pt[:, pr * 128:(pr + 1) * 128], qgT[:, pr, :], S_bd[:, pr, :], True, False, reads=["kgqgT", "S_bd"],
               writes=["b3"], sgc=True)
            for hh in (2 * pr, 2 * pr + 1):
                mm(pt[:, hh * 64:(hh + 1) * 64], MTs[:, hh, :], x_b[:, hh * 64:(hh + 1) * 64], False, hh % 2 == 1,
                   reads=["MTs", "x_b"], writes=["b3"], sgc=True)
        if G7 < 6:
            return
        cp("act", o_f[:].rearrange("p h d -> p (h d)"), pt[:, :], reads=["b3"], writes=["o_f"])
        if G7 < 7:
            return
        for pr in range(4):
            mm(pt[:, pr * 128:(pr + 1) * 128], kd_b[:, pr * 128:(pr + 1) * 128], x_b[:, pr * 128:(pr + 1) * 128],
               True, True, reads=["kd_b", "x_b"], writes=["b3"])
        if G7 < 8:
            return
        for hp in range(2):
            rows = slice(64 * hp, 64 * hp + 64)
            eglh = sm[rows, 48 + hp:56:2]
            tt("dve", S_sb[rows], S_sb[rows], eglh[:, :, None].to_broadcast([64, 4, 64]), ALU.mult,
               reads=["S_sb", "sm_egl"], writes=["S_sb"])
            tt("dve", S_sb[rows], S_sb[rows],
               pt[rows, :].rearrange("p (a d) -> p a d", d=128)[:, :, 64 * hp:64 * hp + 64], ALU.add,
               reads=["S_sb", "b3"], writes=["S_sb"])
            cp("act", S_bd[rows, :, 64 * hp:64 * hp + 64], S_sb[rows], reads=["S_sb"], writes=["S_bd"])
        if G7 < 9:
            return
        act(sq[:, 0:512], o_f[:].rearrange("p h d -> p (h d)"), AF.Square, reads=["o_f"], writes=["cb"])
        if G7 < 10:
            return
        red(sso, sq[:, 0:512].rearrange("p (h d) -> p h d", d=64), ALU.add, reads=["cb"], writes=["sm_sso"])
        if G7 < 11:
            return
        ts("dve", sso, sso, 1.0 / 64, EPS, ALU.mult, ALU.add, reads=["sm_sso"], writes=["sm_sso"])
        if G7 < 12:
            return
        act(sso, sso, AF.Sqrt, reads=["sm_sso"], writes=["sm_sso"])
        if G7 < 13:
            return
        recip(sso, sso, reads=["sm_sso"], writes=["sm_sso"])
        if G7 < 14:
            return
        tt("dve", o_f[:], o_f[:], bc8(sso), ALU.mult, reads=["o_f", "sm_sso"], writes=["o_f"])
        if G7 < 15:
            return
        tt("dve", o_f[:], o_f[:], gaB[:, None, :].to_broadcast([128, 8, 64]), ALU.mult, reads=["o_f", "gaB"],
           writes=["o_f"])
        if G7 < 16:
            return
        tt("dve", o_cat[:, 0:512], o_f[:].rearrange("p h d -> p (h d)"), zs[:], ALU.mult, reads=["o_f", "zs"],
           writes=["o_cat"])
        if G7 < 17:
            return
        if t == NT - 1:
            dma(dl_o, S_sb[:].rearrange("p a d -> p (a d)"), reads=["S_sb"])

    for t in range(NTT):
        rmsnorm_T(t)
        if t < NT:
            gdn_front(t)
        window_qkv(t)
        outputs_kv(t)
        if t < NT:
            ag = attn_prompt(t)
            ng = gdn_prompt(t)
            live = [ng, ag]
            while live:
                for g_ in list(live):
                    try:
                        next(g_)
                    except StopIteration:
                        live.remove(g_)
            attn_done(t)
            gdn_rec(t)
        if t < NT:
            dma(ocat_d[t * 128:(t + 1) * 128, :], o_cat[:], reads=["o_cat"])
        if t >= NT - 1:
            preconv_tm(t)

    R.emit(nc)
    es.close()
    es = ExitStack()

    def sb3(name, shape, dt=F32):
        return es.enter_context(nc.sbuf_tensor(name, list(shape), dt))

    xq = sb3("xq", [16, 1536])
    scv = sb3("scv", [16, 3, 1536])
    wcB = sb3("wcB", [16, 4, 1536])
    cacc = sb3("cacc", [16, 1536])
    ctmp = sb3("ctmp", [16, 1536])
    S128 = sb3("S128", [128, 64, 64])
    Kd = sb3("Kd", [128, 128, 64])
    Vd = sb3("Vd", [128, 128, 64])
    prod = sb3("prod", [128, 128, 64])
    v6 = sb3("v6", [128, 16, 64])
    s1 = sb3("s1", [128, 32])
    pj = sb3("pj", [128, 128])
    ocs = sb3("ocs", [128, 1024], BF)
    o16 = sb3("o16", [16, 1024])

    QC, KC, VC, Z, QR, KR, VW, KS, QS, VN, O, NUM, T1, GA, GB, T2 = [v6[:, i, :] for i in range(16)]
    (a_, b_, alog, dtb, la_, eg_, ssq, ssk, rq, rk, qk_, den, ps_, t_, rno, qkr) = [s1[:, i:i + 1] for i in range(16)]
    W3 = dict(reads=["v6", "s1"], writes=["v6"])
    W1 = dict(reads=["v6", "s1"], writes=["s1"])

    dma(xq[:], pre_s_d, writes=["xq"])
    dma(scv[:], sconv_d, writes=["scv"])
    dma(wcB[:].rearrange("p j c -> p (j c)"), wconv_flat_d.partition_broadcast(16), writes=["wcB"])
    dma(cv_s_o[:, 0:2, :], scv[:, 1:3, :], reads=["scv"])
    dma(cv_s_o[:, 2, :], xq[:], reads=["xq"])
    tt("dve", cacc[:], xq[:], wcB[:, 3, :], ALU.mult, reads=["xq", "wcB"], writes=["cacc"])
    for j in range(3):
        tt("pool", ctmp[:], scv[:, j, :], wcB[:, j, :], ALU.mult, reads=["scv", "wcB"], writes=["ctmp"])
        tt("dve", cacc[:], cacc[:], ctmp[:], ALU.add, reads=["cacc", "ctmp"], writes=["cacc"])
    act(cacc[:], cacc[:], AF.Silu, reads=["cacc"], writes=["cacc"])
    for j in range(3):
        dma(c3_d[j], cacc[:, j * 512:(j + 1) * 512], reads=["cacc"], writes=["c3_d"])
    for j, dst in enumerate((QC, KC, VC)):
        dma(dst, c3_d[j].rearrange("n (h d) -> (n h) d", d=64), reads=["c3_d"], writes=["v6"])
    dma(Z, z_s_d.rearrange("n (h d) -> (n h) d", d=64), writes=["v6"])
    dma(QR, qk_s_d[0].rearrange("n (h d) -> (n h) d", d=64), writes=["v6"])
    dma(KR, qk_s_d[1].rearrange("n (h d) -> (n h) d", d=64), writes=["v6"])
    dma(VW, v_s_d.rearrange("n (h d) -> (n h) d", d=64), writes=["v6"])
    dma(GA, g_a_d.partition_broadcast(128), writes=["v6"])
    dma(GB, g_b_d.partition_broadcast(128), writes=["v6"])
    dma(a_, ab_s_d[0].rearrange("n (h o) -> (n h) o", o=1), writes=["s1"])
    dma(b_, ab_s_d[1].rearrange("n (h o) -> (n h) o", o=1), writes=["s1"])
    dma(alog, alog128_d, writes=["s1"])
    dma(dtb, dtb128_d, writes=["s1"])
    dma(S128[:].rearrange("p a b -> p (a b)"), sdelta_d.rearrange("n h a b -> (n h) (a b)"), writes=["S128"])
    tt("dve", a_, a_, dtb, ALU.add, **W1)
    act(a_, a_, AF.Exp, **W1)
    act(a_, a_, AF.Ln, bias=1.0, **W1)
    act(alog, alog, AF.Exp, **W1)
    tt("dve", la_, a_, alog, ALU.mult, **W1)
    act(eg_, la_, AF.Exp, scale=-1.0, **W1)
    act(b_, b_, AF.Sigmoid, **W1)
    for src, ssx, rx in ((QC, ssq, rq), (KC, ssk, rk)):
        tt("dve", T1, src, src, ALU.mult, **W3)
        red(ssx, T1, ALU.add, **W1)
        ts("dve", ssx, ssx, EPS, None, ALU.add, **W1)
        act(ssx, ssx, AF.Sqrt, **W1)
        recip(rx, ssx, **W1)
        ts("dve", src, src, rx, None, ALU.mult, **W3)
    for src, dst in ((KC, KS), (QC, QS)):
        tt("dve", prod[:, 0:64, :], S128[:], src[:, :, None].to_broadcast([128, 64, 64]), ALU.mult,
           reads=["S128", "v6"], writes=["prod"])
        red(dst, prod[:, 0:64, :].rearrange("p a b -> p b a"), ALU.add, reads=["prod"], writes=["v6"])
    ts("dve", T1, KS, eg_, None, ALU.mult, **W3)
    tt("dve", T1, VC, T1, ALU.subtract, **W3)
    ts("dve", VN, T1, b_, None, ALU.mult, **W3)
    tt("dve", T1, QC, KC, ALU.mult, **W3)
    red(qk_, T1, ALU.add, **W1)
    ts("dve", O, QS, eg_, None, ALU.mult, **W3)
    stt(O, VN, qk_, O, ALU.mult, ALU.add, **W3)
    ts("dve", O, O, 0.125, None, ALU.mult, **W3)
    ts("dve", S128[:].rearrange("p a b -> p (a b)"), S128[:].rearrange("p a b -> p (a b)"), eg_, None, ALU.mult,
       reads=["S128", "s1"], writes=["S128"])
    tt("pool", prod[:, 0:64, :], KC[:, :, None].to_broadcast([128, 64, 64]), VN[:, None, :].to_broadcast([128, 64, 64]),
       ALU.mult, reads=["v6"], writes=["prod"])
    tt("dve", S128[:], S128[:], prod[:, 0:64, :], ALU.add, reads=["S128", "prod"], writes=["S128"])
    dma(dl_s_o.rearrange("n h a b -> (n h) (a b)"), S128[:].rearrange("p a b -> p (a b)"), reads=["S128"])
    def rms64(x_, g_vec, outp):
        tt("dve", T1, x_, x_, ALU.mult, **W3)
        red(rno, T1, ALU.add, **W1)
        ts("dve", rno, rno, 1.0 / 64, EPS, ALU.mult, ALU.add, **W1)
        act(rno, rno, AF.Sqrt, **W1)
        recip(rno, rno, **W1)
        ts("dve", x_, x_, rno, None, ALU.mult, **W3)
        tt("dve", outp, x_, g_vec, ALU.mult, **W3)
    rms64(O, GA, O)
    act(Z, Z, AF.Silu, **W3)
    tt("dve", O, O, Z, ALU.mult, **W3)
    dma(oab_d[0].rearrange("n (h d) -> (n h) d", d=64), O, reads=["v6"], writes=["oab_d"])
    R.op("pool", lambda e: e.memset(NUM, 0.0), writes=["v6n"])
    R.op("pool", lambda e: e.memset(den, 0.0), writes=["s1d"])
    for di, dil in enumerate((1, 4, 16)):
        st = 2048 - 128 * dil
        for n_ in range(16):
            dma(Kd[n_ * 8:(n_ + 1) * 8], ck_d[n_, st:2048:dil, :, :].rearrange("j h d -> h j d"), writes=["Kd"])
            dma(Vd[n_ * 8:(n_ + 1) * 8], cv_d[n_, st:2048:dil, :, :].rearrange("j h d -> h j d"), writes=["Vd"])
        tt("dve", prod[:], Kd[:], QR[:, None, :].to_broadcast([128, 128, 64]), ALU.mult, reads=["Kd", "v6"],
           writes=["prod"])
        red(pj[:], prod[:], ALU.add, reads=["prod"], writes=["pj"])
        act(pj[:], pj[:], AF.Exp, scale=0.125, reads=["pj"], writes=["pj"])
        red(t_, pj[:], ALU.add, reads=["pj"], writes=["s1t"])
        tt("dve", den, den, t_, ALU.add, reads=["s1d", "s1t"], writes=["s1d"])
        tt("pool", prod[:], Vd[:], pj[:, :, None].to_broadcast([128, 128, 64]), ALU.mult, reads=["Vd", "pj"],
           writes=["prod"])
        red(T2, prod[:].rearrange("p j d -> p d j"), ALU.add, reads=["prod"], writes=["v6t2"])
        tt("dve", NUM, NUM, T2, ALU.add, reads=["v6n", "v6t2"], writes=["v6n"])
    tt("dve", T1, QR, KR, ALU.mult, **W3)
    red(ps_, T1, ALU.add, **W1)
    act(ps_, ps_, AF.Exp, scale=0.125, **W1)
    ts("dve", ps_, ps_, 3.0, None, ALU.mult, **W1)
    tt("dve", den, den, ps_, ALU.add, reads=["s1", "s1d"], writes=["s1d"])
    stt(NUM, VW, ps_, NUM, ALU.mult, ALU.add, reads=["v6", "s1", "v6n"], writes=["v6n"])
    recip(den, den, reads=["s1d"], writes=["s1d"])
    ts("dve", NUM, NUM, den, None, ALU.mult, reads=["v6n", "s1d"], writes=["v6"])
    rms64(NUM, GB, NUM)
    dma(oab_d[1].rearrange("n (h d) -> (n h) d", d=64), NUM, reads=["v6"], writes=["oab_d"])
    dma(o16[:, 0:512], oab_d[0], reads=["oab_d"], writes=["o16"])
    dma(o16[:, 512:1024], oab_d[1], reads=["oab_d"], writes=["o16"])
    R.op("pool", lambda e: e.memset(ocs[:], 0.0), writes=["ocs"])
    cp("act", ocs[0:16, :], o16[:], reads=["o16", "ocs"], writes=["ocs"])
    dma(ocat_d[NT * 128:(NT + 1) * 128, :], ocs[:], reads=["ocs"])
    R.emit(nc)
    es.close()
    es = ExitStack()
    def sb2(name, shape, dt=F32):
        return es.enter_context(nc.sbuf_tensor(name, list(shape), dt))

    BS = 256
    NB = (2 * ROWS) // BS + 32
    SUB = BS // 128
    w_out = sb2("w_out_sb", [128, 8, 1024], BF)
    wpg = sb2("wpg_sb", [128, 8, 1024], BF)
    wpp = sb2("wpp_sb", [128, 2, 1024], BF)
    w_rt = sb2("w_rt_sb", [128, 8, 36])
    brtB = sb2("brtB", [128, 36])
    gffnB = sb2("gffnB", [128, 1024])
    gfinB = sb2("gfinB", [128, 1024])
    ident_f2 = sb2("ident_f2", [128, 128])
    ident_b2 = sb2("ident_b2", [128, 128], BF)
    triS = sb2("triS_sb", [128, 128])
    ones2 = sb2("ones2", [128, 128])
    B128 = sb2("B128", [128, NB])
    Wg = [sb2("Wg%d" % i, [128, 8, 512], BF) for i in range(2)]
    Wu = [sb2("Wu%d" % i, [128, 8, 512], BF) for i in range(2)]
    Wd = [sb2("Wd%d" % i, [128, 4, 1024], BF) for i in range(2)]
    h1t = sb2("h1t", [128, 1024])
    m_f = sb2("m_f", [128, 1024])
    m_b = sb2("m_b", [128, 1024], BF)
    sq2 = sb2("sq2", [128, 1024])
    oc_sb = sb2("oc_sb", [128, 1024], BF)
    ocT = sb2("ocT", [128, 8, 128], BF)
    xt2 = [sb2("xt2_%d" % i, [128, 1024]) for i in range(2)]
    p_sb = sb2("p_sb", [128, 256])
    p_b = sb2("p_b", [128, 256], BF)
    pT = sb2("pT", [128, 2, 128], BF)
    mTf = sb2("mTf", [128, 8, 128])
    h2b = sb2("h2b", [128, 1024], BF)
    h2T = sb2("h2T", [128, 8, 128], BF)
    sig = sb2("sig", [128, 1024])
    rs = sb2("rs", [128, 128])
    st2 = sb2("st2", [128, 4])
    OHs = sb2("OHs", [128, NTT, 64])
    rnk = sb2("rnk", [128, NTT, 2])
    gts = sb2("gts", [128, NTT, 2])
    dstF = sb2("dstF", [128, NTT, 2])
    dstI = sb2("dstI", [128, NTT, 2], I32)
    carry = sb2("carry", [128, 32])
    r32 = sb2("r32", [128, 8, 32])
    cmpb = sb2("cmpb", [128, NB, 32])
    E_f = sb2("E_f", [128, NB])
    idxW = sb2("idxW", [128, NB], I32)
    pidx = sb2("pidx_sb", [128, 1])
    xb = [sb2("xb%d" % i, [128, 1024], BF) for i in range(2)]
    xbT = sb2("xbT", [128, 8, 128], BF)
    sgt = sb2("sgt", [128, 512])
    hb = sb2("hb", [128, 512], BF)
    hbT = sb2("hbT", [128, 4, 128], BF)
    yb = [sb2("yb%d" % i, [128, 1024]) for i in range(2)]
    yg = [sb2("yg%d" % i, [128, 1024]) for i in range(2)]
    zt = sb2("zt", [128, 1024], BF)
    bk = [pw[:, 0:512], pw[:, 512:1024], pw[:, 1024:1536], pt[:, :], ps2[:, 0:512], ps2[:, 512:1024],
          po2[:, 0:512], po2[:, 512:1024]]
    h1_d = nc.dram_tensor("h1_scr", [ROWS, 1024], F32, kind="Internal").ap()
    m_d = nc.dram_tensor("m_scr", [ROWS, 1024], BF, kind="Internal").ap()
    xbuf_d = nc.dram_tensor("xbuf_scr", [NB * BS, 1024], BF, kind="Internal").ap()
    ybuf_d = nc.dram_tensor("ybuf_scr", [NB * BS, 1024], F32, kind="Internal").ap()

    dma(ident_f2[:], ident_f_d, writes=["ident_f"])
    dma(ident_b2[:], ident_b_d, writes=["ident_b"])
    dma(triS[:], tris_d, writes=["triS"])
    dma(pidx[:], pidx_d, writes=["pidx"])
    dma(ones2[:], ones_d, writes=["ones2"])
    dma(B128[:], b128_d.partition_broadcast(128), writes=["B128"])
    dma(gffnB[:], g_ffn_d.partition_broadcast(128), writes=["gffnB"])
    dma(gfinB[:], g_fin_d.partition_broadcast(128), writes=["gfinB"])
    dma(brtB[:], b_rt_d.partition_broadcast(128), writes=["brtB"])
    dma(w_rt[:], w_rt_d.rearrange("(k p) n -> p k n", p=128), writes=["w_rt"])
    for k in range(8):
        dma(w_out[:, k, :], w_out_d.rearrange("(k p) n -> p k n", p=128)[:, k, :], writes=["w_out"], eng="pool")
        dma(wpg[:, k, :], wpg_d.rearrange("(k p) n -> p k n", p=128)[:, k, :], writes=["wpg"], eng="pool")
    for k in range(2):
        dma(wpp[:, k, :], wpp_d.rearrange("(k p) n -> p k n", p=128)[:, k, :], writes=["wpp"], eng="pool")
    R.op("pool", lambda e: e.memset(carry[:], 0.0), writes=["carry"])
    R.op("pool", lambda e: e.memset(zt[:], 0.0), writes=["zt"])
    for b in range(NB * SUB):
        dma(xbuf_d[b * 128:(b + 1) * 128, :], zt[:], reads=["zt"], writes=["xbuf_z"], eng="pool")

    def rms2(src, gB_, dst, tag):
        act(sq2[:], src, AF.Square, reads=[tag], writes=["sq2"])
        red(st2[:, 0:1], sq2[:], ALU.add, reads=["sq2"], writes=["st2"])
        ts("dve", st2[:, 0:1], st2[:, 0:1], 1.0 / D, EPS, ALU.mult, ALU.add, reads=["st2"], writes=["st2"])
        act(st2[:, 0:1], st2[:, 0:1], AF.Sqrt, reads=["st2"], writes=["st2"])
        recip(st2[:, 1:2], st2[:, 0:1], reads=["st2"], writes=["st2"])
        stt(dst, src, st2[:, 1:2], gB_[:], ALU.mult, ALU.mult, reads=[tag, "st2", "gffnB", "gfinB"], writes=["dst_" + tag])

    def routing(t):
        lg = rs[:, 0:36]
        tt("dve", lg, pt[:, 0:36], brtB[:], ALU.add, reads=["b3", "brtB"], writes=["rs"])
        gl4 = rs[:, 0:4]
        el = rs[:, 4:36].rearrange("p (g j) -> p g j", j=8)
        gmax, ngmax, gsum, pgrp = rs[:, 36:37], rs[:, 37:38], rs[:, 38:39], rs[:, 39:40]
        goh, gex = rs[:, 40:44], rs[:, 44:48]
        tmp = rs[:, 48:80].rearrange("p (g j) -> p g j", j=8)
        e_in, oh1, e2, oh2 = rs[:, 80:88], rs[:, 88:96], rs[:, 96:104], rs[:, 104:112]
        m1, m2, d21, w1, w2 = rs[:, 120:121], rs[:, 121:122], rs[:, 122:123], rs[:, 123:124], rs[:, 124:125]
        RW = dict(reads=["rs"], writes=["rs"])
        red(gmax, gl4, ALU.max, **RW)
        ts("dve", ngmax, gmax, -1.0, None, ALU.mult, **RW)
        ts("dve", goh, gl4, gmax, None, ALU.is_equal, **RW)
        act(gex, gl4, AF.Exp, bias=ngmax, **RW)
        red(gsum, gex, ALU.add, **RW)
        recip(pgrp, gsum, **RW)
        tt("dve", tmp, el, goh[:, :, None].to_broadcast([128, 4, 8]), ALU.mult, **RW)
        red(e_in, tmp.rearrange("p g j -> p j g"), ALU.add, **RW)
        red(m1, e_in, ALU.max, **RW)
        ts("dve", oh1, e_in, m1, None, ALU.is_equal, **RW)
        stt(e2, oh1, -1e30, e_in, ALU.mult, ALU.add, **RW)
        red(m2, e2, ALU.max, **RW)
        ts("dve", oh2, e2, m2, None, ALU.is_equal, **RW)
        tt("dve", d21, m2, m1, ALU.subtract, **RW)
        act(d21, d21, AF.Exp, **RW)
        ts("dve", w1, d21, 1.0, None, ALU.add, **RW)
        recip(w1, w1, **RW)
        tt("dve", w2, d21, w1, ALU.mult, **RW)
        tt("dve", gts[:, t, 0:1], w1, pgrp, ALU.mult, reads=["rs"], writes=["gts"])
        tt("dve", gts[:, t, 1:2], w2, pgrp, ALU.mult, reads=["rs"], writes=["gts"])
        for k_, ohk in ((0, oh1), (1, oh2)):
            tt("dve", OHs[:, t, 32 * k_:32 * k_ + 32].rearrange("p (g j) -> p g j", j=8),
               goh[:, :, None].to_broadcast([128, 4, 8]), ohk[:, None, :].to_broadcast([128, 4, 8]), ALU.mult,
               reads=["rs"], writes=["OHs"])
        oh12, cc, tm = r32[:, 0, :], r32[:, 1, :], r32[:, 2, :]
        tt("dve", oh12, OHs[:, t, 0:32], OHs[:, t, 32:64], ALU.add, reads=["OHs"], writes=["r32a"])
        mm(pt[:, 64:96], triS[:], oh12, True, True, reads=["triS", "r32a"], writes=["b3"])
        mm(pt[:, 96:128], ones2[:], oh12, True, True, reads=["ones2", "r32a"], writes=["b3"])
        tt("dve", cc, pt[:, 64:96], carry[:], ALU.add, reads=["b3", "carry"], writes=["r32b"])
        for k_ in range(2):
            tt("dve", tm, OHs[:, t, 32 * k_:32 * k_ + 32], cc, ALU.mult, reads=["OHs", "r32b"], writes=["r32c"])
            red(rnk[:, t, k_:k_ + 1], tm, ALU.add, reads=["r32c"], writes=["rnk"])
        tt("dve", carry[:], carry[:], pt[:, 96:128], ALU.add, reads=["carry", "b3"], writes=["carry"])

    pTb = pt[:, :].bitcast(BF)
    for t in range(NTT):
        xs = t % 2
        dma(oc_sb[:], ocat_d[t * 128:(t + 1) * 128, :], writes=["oc_sb"])
        dma(xt2[xs][:], x_d[t * 128:(t + 1) * 128, :], writes=["xt2_%d" % xs])
        for k in range(8):
            tr(pTb[:, k * 128:(k + 1) * 128], oc_sb[:, k * 128:(k + 1) * 128], ident_b2[:],
               reads=["oc_sb", "ident_b"], writes=["b3"])
        cp("act", ocT[:].rearrange("p k t -> p (k t)"), pTb, reads=["b3"], writes=["ocT"])
        for half in range(2):
            for k in range(8):
                mm(bk[4 + half], ocT[:, k, :], w_out[:, k, half * 512:(half + 1) * 512], k == 0, k == 7,
                   reads=["ocT", "w_out"], writes=["b%d" % (4 + half)])
        tt("dve", h1t[:], ps2[:, :], xt2[xs][:], ALU.add, reads=["b4", "b5", "xt2_%d" % xs], writes=["h1"])
        dma(h1_d[t * 128:(t + 1) * 128, :], h1t[:], reads=["h1"], writes=["h1_d"])
        rms2(h1t[:], gffnB, m_f[:], "h1")
        cp("act", m_b[:], m_f[:], reads=["dst_h1"], writes=["m_b"])
        dma(m_d[t * 128:(t + 1) * 128, :], m_b[:], reads=["m_b"], writes=["m_d"])
        for k in range(8):
            tr(po2[:, k * 128:(k + 1) * 128], m_f[:, k * 128:(k + 1) * 128], ident_f2[:],
               reads=["dst_h1", "ident_f"], writes=["b6", "b7"])
        cp("act", mTf[:].rearrange("p k t -> p (k t)"), po2[:, :], reads=["b6", "b7"], writes=["mTf"])
        for k in range(8):
            mm(pt[:, 0:36], mTf[:, k, :], w_rt[:, k, :], k == 0, k == 7, reads=["mTf", "w_rt"], writes=["b3"])
        routing(t)
    nblk, padded, pa, pb_, pstart = r32[:, 3, :], r32[:, 4, :], r32[:, 5, :], r32[:, 6, :], r32[:, 7, :]
    NJ = NB
    tt("dve", cmpb[:, 0:NJ, :].rearrange("p j e -> p e j"), carry[:, :, None].to_broadcast([128, 32, NJ]),
       B128[:, None, 0:NJ].to_broadcast([128, 32, NJ]), ALU.is_gt, reads=["carry", "B128"], writes=["cmpb"])
    red(nblk, cmpb[:, 0:NJ, :].rearrange("p j e -> p e j"), ALU.add, reads=["cmpb"], writes=["r32d"])
    ts("dve", padded, nblk, float(BS), None, ALU.mult, reads=["r32d"], writes=["r32e"])
    cp("pool", pa, padded, reads=["r32e"], writes=["r32f"])
    src_, dst_ = pa, pb_
    sn, dn = "r32f", "r32g"
    for s_ in (1, 2, 4, 8, 16):
        cp("pool", dst_[:, 0:s_], src_[:, 0:s_], reads=[sn], writes=[dn])
        tt("dve", dst_[:, s_:32], src_[:, s_:32], src_[:, 0:32 - s_], ALU.add, reads=[sn], writes=[dn])
        src_, dst_ = dst_, src_
        sn, dn = dn, sn
    pend = src_
    tt("dve", pstart, pend, padded, ALU.subtract, reads=[sn, "r32e"], writes=["r32h"])
    tt("dve", cmpb[:], pend[:, None, :].to_broadcast([128, NB, 32]), B128[:, :, None].to_broadcast([128, NB, 32]),
       ALU.is_le, reads=[sn, "B128", "cmpb"], writes=["cmpb"])
    red(E_f[:], cmpb[:], ALU.add, reads=["cmpb"], writes=["E_f"])
    ts("dve", E_f[:], E_f[:], 31.0, None, ALU.min, reads=["E_f"], writes=["E_f"])
    ts("dve", E_f[:], E_f[:], 128.0, pidx[:, 0:1], ALU.mult, ALU.add, reads=["E_f", "pidx"], writes=["E_f"])
    cp("dve", idxW[:], E_f[:], reads=["E_f"], writes=["idxW"])
    for t in range(NTT):
        for k_ in range(2):
            tm = r32[:, k_, :]
            tt("dve", tm, OHs[:, t, 32 * k_:32 * k_ + 32], pstart, ALU.mult, reads=["OHs", "r32h"], writes=["r32t%d" % k_])
            red(dstF[:, t, k_:k_ + 1], tm, ALU.add, reads=["r32t%d" % k_], writes=["dstF"])
        tt("dve", dstF[:, t, :], dstF[:, t, :], rnk[:, t, :], ALU.add, reads=["dstF", "rnk"], writes=["dstF"])
        cp("dve", dstI[:, t, :], dstF[:, t, :], reads=["dstF"], writes=["dstI"])
        xs = t % 2
        dma(xb[xs][:], m_d[t * 128:(t + 1) * 128, :], reads=["m_d"], writes=["xb%d" % xs])
        for k_ in range(2):
            R.op("pool", (lambda xs=xs, t=t, k_=k_: (lambda e: e.indirect_dma_start(
                out=xbuf_d[:, :], out_offset=bass.IndirectOffsetOnAxis(ap=dstI[:, t, k_:k_ + 1], axis=0),
                in_=xb[xs][:], in_offset=None)))(), reads=["xb%d" % xs, "dstI", "xbuf_z"], writes=["xbuf"], dma=True)
    for b in range(NB):
        s_ = b % 2
        for wt, wsrc, nm in ((Wg, wg_bf, "Wg"), (Wu, wu_bf, "Wu"), (Wd, wd_bf, "Wd")):
            R.op("pool", (lambda wt=wt, wsrc=wsrc, b=b, s_=s_: (lambda e: e.indirect_dma_start(
                out=wt[s_][:].rearrange("p a n -> p (a n)"), out_offset=None, in_=wsrc[:, :],
                in_offset=bass.IndirectOffsetOnAxis(ap=idxW[:, b:b + 1], axis=0))))(),
                reads=["idxW"], writes=["%s%d" % (nm, s_)], dma=True)
        for sub in range(SUB):
            q_ = (b * SUB + sub) % 2
            r0 = b * BS + sub * 128
            dma(xb[q_][:], xbuf_d[r0:r0 + 128, :], reads=["xbuf"], writes=["xb%d" % q_])
            for k in range(8):
                tr(pTb[:, k * 128:(k + 1) * 128], xb[q_][:, k * 128:(k + 1) * 128], ident_b2[:],
                   reads=["xb%d" % q_, "ident_b"], writes=["b3"])
            cp("act", xbT[:].rearrange("p k t -> p (k t)"), pTb, reads=["b3"], writes=["xbT"])
            for k in range(8):
                mm(bk[4], xbT[:, k, :], Wg[s_][:, k, :], k == 0, k == 7, reads=["xbT", "Wg%d" % s_], writes=["b4"])
            for k in range(8):
                mm(bk[5], xbT[:, k, :], Wu[s_][:, k, :], k == 0, k == 7, reads=["xbT", "Wu%d" % s_], writes=["b5"])
            act(sgt[:], bk[4], AF.Silu, reads=["b4"], writes=["sgt"])
            tt("dve", hb[:], sgt[:], bk[5], ALU.mult, reads=["sgt", "b5"], writes=["hb"])
            for c in range(4):
                tr(pTb[:, c * 128:(c + 1) * 128], hb[:, c * 128:(c + 1) * 128], ident_b2[:], reads=["hb", "ident_b"],
                   writes=["b3"])
            cp("act", hbT[:].rearrange("p c t -> p (c t)"), pTb[:, 0:512], reads=["b3"], writes=["hbT"])
            for half in range(2):
                for c in range(4):
                    mm(bk[6 + half], hbT[:, c, :], Wd[s_][:, c, half * 512:(half + 1) * 512], c == 0, c == 3,
                       reads=["hbT", "Wd%d" % s_], writes=["b%d" % (6 + half)])
            cp("act", yb[q_][:], po2[:, :], reads=["b6", "b7"], writes=["yb%d" % q_])
            dma(ybuf_d[r0:r0 + 128, :], yb[q_][:], reads=["yb%d" % q_], writes=["ybuf"])
    for t in range(NTT):
        xs = t % 2
        dma(h1t[:], h1_d[t * 128:(t + 1) * 128, :], reads=["h1_d"], writes=["h1"])
        dma(p_sb[:], p_d[t * 128:(t + 1) * 128, :], writes=["p_sb"])
        for k_ in range(2):
            R.op("pool", (lambda t=t, k_=k_: (lambda e: e.indirect_dma_start(
                out=yg[k_][:], out_offset=None, in_=ybuf_d[:, :],
                in_offset=bass.IndirectOffsetOnAxis(ap=dstI[:, t, k_:k_ + 1], axis=0))))(),
                reads=["ybuf", "dstI"], writes=["yg%d" % k_], dma=True)
            stt(h1t[:], yg[k_][:], gts[:, t, k_:k_ + 1], h1t[:], ALU.mult, ALU.add, reads=["yg%d" % k_, "gts", "h1"],
                writes=["h1"])
        cp("act", h2b[:], h1t[:], reads=["h1"], writes=["h2b"])
        for k in range(8):
            tr(pTb[:, k * 128:(k + 1) * 128], h2b[:, k * 128:(k + 1) * 128], ident_b2[:], reads=["h2b", "ident_b"],
               writes=["b3"])
        cp("act", h2T[:].rearrange("p k t -> p (k t)"), pTb, reads=["b3"], writes=["h2T"])
        for half in range(2):
            for k in range(8):
                mm(bk[4 + half], h2T[:, k, :], wpg[:, k, half * 512:(half + 1) * 512], k == 0, k == 7,
                   reads=["h2T", "wpg"], writes=["b%d" % (4 + half)])
        act(sig[:], ps2[:, :], AF.Sigmoid, reads=["b4", "b5"], writes=["sig"])
        cp("act", p_b[:], p_sb[:], reads=["p_sb"], writes=["p_b"])
        for k in range(2):
            tr(pTb[:, k * 128:(k + 1) * 128], p_b[:, k * 128:(k + 1) * 128], ident_b2[:], reads=["p_b", "ident_b"],
               writes=["b3"])
        cp("act", pT[:].rearrange("p k t -> p (k t)"), pTb[:, 0:256], reads=["b3"], writes=["pT"])
        for half in range(2):
            for k in range(2):
                mm(bk[6 + half], pT[:, k, :], wpp[:, k, half * 512:(half + 1) * 512], k == 0, k == 1,
                   reads=["pT", "wpp"], writes=["b%d" % (6 + half)])
        tt("dve", sig[:], sig[:], po2[:, :], ALU.mult, reads=["sig", "b6", "b7"], writes=["sig"])
        tt("dve", h1t[:], h1t[:], sig[:], ALU.add, reads=["h1", "sig"], writes=["h1"])
        rms2(h1t[:], gfinB, m_f[:], "h1")
        dma(y_d[t * 128:(t + 1) * 128, :], m_f[:], reads=["dst_h1"])
    R.emit(nc)
    es.close()
    esg.close()
    return nc


_CACHE = {}


def _cmask():
    k = np.arange(128)[:, None, None]
    dl = np.arange(NWIN)[None, :, None]
    q = np.arange(128)[None, None, :]
    d = 128 * dl + q - k
    c = ((d <= 128).astype(np.float32) + ((d % 4 == 0) & (d <= 512)) + ((d % 16 == 0) & (d <= 2048))) * (d >= 0)
    return c.reshape(128, NWIN * 128).astype(ml_dtypes.bfloat16)


def _consts(NT):
    NTT = NT + 1
    pos = np.concatenate([np.arange(NT * 128, dtype=np.float32), np.full(128, 8192.0, np.float32)])
    half = 8
    inv = (np.float32(500000.0) ** (-np.arange(half, dtype=np.float32) * np.float32(2.0 / 16))).astype(np.float32)
    ang = (pos[:, None] * inv[None, :]).astype(np.float32)
    return {
        "cos_t": np.cos(ang).astype(np.float32),
        "sin_t": np.sin(ang).astype(np.float32),
        "ident_f": np.eye(128, dtype=np.float32),
        "ident_b": np.eye(128).astype(ml_dtypes.bfloat16),
        "cmask": _cmask(),
        "triF": np.triu(np.ones((128, 128), np.float32)),
        "onesF": np.ones((128, 128), np.float32),
        "triS": np.triu(np.ones((128, 128), np.float32), 1),
        "pidx": np.arange(128, dtype=np.float32)[:, None],
        "b128": (256.0 * np.arange((2 * (NT + 1) * 128) // 256 + 32, dtype=np.float32))[None, :],
        "triN": -np.triu(np.ones((128, 128), np.float32)),
        "maskUi": np.where(np.arange(128)[None, :] >= np.arange(128)[:, None], 0.0, -30000.0).astype(np.float32),
        "maskUs": np.where(np.arange(128)[None, :] > np.arange(128)[:, None], 0.0, -30000.0).astype(np.float32),
        "maskLs": np.where(np.arange(128)[None, :] < np.arange(128)[:, None], 0.0, -30000.0).astype(np.float32),
    }


def run(inputs, NT, n_cores, KEEP):
    key = (NT, KEEP)
    if key not in _CACHE:
        _CACHE[key] = build(NT, KEEP)
    nc = _CACHE[key]
    S = NT * 128
    consts = _consts(NT)
    in_maps = []
    for c in range(n_cores):
        xs = np.zeros((128, D), np.float32)
        xs[0:16] = inputs["x_sample"][16 * c:16 * c + 16, 0]
        x = np.concatenate([inputs["x_prompt"][c], xs], axis=0)
        m = {"x": np.ascontiguousarray(x),
             "w_in": np.ascontiguousarray(inputs["w_in"][0]),
             "g_attn": np.ascontiguousarray(inputs["g_attn_norm"][0][None, :]),
             "g_b": np.ascontiguousarray(inputs["g_b_out"][0][None, :]),
             "g_a": np.ascontiguousarray(inputs["g_a_out"][0][None, :]),
             "w_out": np.ascontiguousarray(inputs["w_out"][0]),
             "state_conv": np.ascontiguousarray(inputs["state_conv"][0, 16 * c:16 * c + 16]),
             "state_delta": np.ascontiguousarray(inputs["state_delta"][0, 16 * c:16 * c + 16]),
             "cache_k": np.ascontiguousarray(inputs["cache_win_k"][0, 16 * c:16 * c + 16]),
             "cache_v": np.ascontiguousarray(inputs["cache_win_v"][0, 16 * c:16 * c + 16]),
             "wconv_flat": np.ascontiguousarray(inputs["w_conv"][0].reshape(1, -1)),
             "alog128": np.ascontiguousarray(np.tile(inputs["a_log"][0], 16)[:, None]),
             "dtb128": np.ascontiguousarray(np.tile(inputs["dt_bias"][0], 16)[:, None]),
             "g_ffn": np.ascontiguousarray(inputs["g_ffn_norm"][0][None, :]),
             "g_fin": np.ascontiguousarray(inputs["g_final"][None, :]),
             "w_rt": np.ascontiguousarray(np.concatenate(
                 [inputs["w_router_group"][0], inputs["w_router_expert"][0].reshape(D, 32)], axis=1)),
             "b_rt": np.ascontiguousarray(np.concatenate(
                 [inputs["b_router_group"][0], inputs["b_router_expert"][0].reshape(32)])[None, :]),
             "w_eg": np.ascontiguousarray(inputs["w_exp_gate"][0]),
             "w_eu": np.ascontiguousarray(inputs["w_exp_up"][0]),
             "w_ed": np.ascontiguousarray(inputs["w_exp_down"][0]),
             "w_pg": np.ascontiguousarray(inputs["w_ple_gate"][0]),
             "w_pp": np.ascontiguousarray(inputs["w_ple_proj"][0]),
             "p_all": np.ascontiguousarray(np.concatenate(
                 [inputs["p_prompt"][0, c], np.concatenate([inputs["p_sample"][0, 16 * c:16 * c + 16, 0],
                                                            np.zeros((112, 256), np.float32)])], axis=0)),
             "a_log": np.ascontiguousarray(inputs["a_log"][0][None, :]),
             "dt_bias": np.ascontiguousarray(inputs["dt_bias"][0][None, :]),
             "wconvT": np.ascontiguousarray(
                 inputs["w_conv"][0].T.reshape(12, 128, 4).transpose(1, 0, 2).reshape(128, 48))}
        m.update(consts)
        in_maps.append(m)
    res = run_bass_kernel_spmd(nc, in_maps, core_ids=list(range(n_cores)))
    return res.results


def kernel(**inputs):
    inputs = {k: np.asarray(v) for k, v in inputs.items()}
    NT, KEEP, n = 64, 2048, 8
    r = run(inputs, NT, n, KEEP)
    NP, S = 8, 8192
    y_p = np.stack([r[c]["y"][:S] for c in range(n)])
    y_s = np.concatenate([r[c]["y"][S:S + 16] for c in range(n)])[:, None, :]
    wk_p = np.stack([r[c]["wk_p"].reshape(KEEP, 8, 64) for c in range(n)])[None]
    wv_p = np.stack([r[c]["wv_p"].reshape(KEEP, 8, 64) for c in range(n)])[None]
    cv_p = np.stack([r[c]["cv_p"] for c in range(n)])[None]
    dl_p = np.stack([r[c]["dl_p"].reshape(2, 64, 4, 64).transpose(2, 0, 1, 3).reshape(8, 64, 64) for c in range(n)])[None]
    wk_s = np.concatenate([r[c]["wk_s"].reshape(16, 1, 8, 64) for c in range(n)])[None]
    wv_s = np.concatenate([r[c]["wv_s"].reshape(16, 1, 8, 64) for c in range(n)])[None]
    cv_s = np.concatenate([r[c]["cv_s"] for c in range(n)])[None]
    dl_s = np.concatenate([r[c]["dl_s"] for c in range(n)])[None]
    return (y_p.astype(np.float32), y_s.astype(np.float32), wk_p.astype(np.float32), wv_p.astype(np.float32), cv_p.astype(np.float32), dl_p.astype(np.float32),
            wk_s.astype(np.float32), wv_s.astype(np.float32), cv_s.astype(np.float32), dl_s.astype(np.float32))
```

```python
import math
import os
from contextlib import ExitStack

import numpy as np
import ml_dtypes
import concourse.bass as bass
import concourse.mybir as mybir
from concourse.bass_utils import run_bass_kernel_spmd

F32 = mybir.dt.float32
BF = mybir.dt.bfloat16
I32 = mybir.dt.int32
AF = mybir.ActivationFunctionType
ALU = mybir.AluOpType
AX = mybir.AxisListType

ENGS = ("pe", "act", "dve", "pool", "sp")

D = 1024
HD = 64
NH = 8
IN_W = 3600
OFF_Z = 1536
OFF_A = 2048
OFF_B = 2056
OFF_WIN = 2064
EPS = 1e-6
NWIN = 17


class Op:
    __slots__ = ("eng", "fn", "deps", "is_dma", "sig_idx", "idx", "dma_sem", "dma_val")

    def __init__(self, eng, fn, deps, is_dma):
        self.eng = eng
        self.fn = fn
        self.deps = deps
        self.is_dma = is_dma
        self.sig_idx = None
        self.dma_sem = None
        self.dma_val = None


class Rec:
    def __init__(self):
        self.ops = {e: [] for e in ENGS}
        self.last_w = {}
        self.readers = {}
        self.n_dma_sems = 16

    EXPAND = {"ps2": ("ps2a", "ps2b"), "po2": ("po2a", "po2b"), "ca": ("ca_l", "ca_h"), "cb": ("cb_l", "cb_h")}

    def op(self, eng, fn, reads=(), writes=(), dma=False):
        reads = [x for b in reads for x in self.EXPAND.get(b, (b,))]
        writes = [x for b in writes for x in self.EXPAND.get(b, (b,))]
        deps = []
        for b in reads:
            w = self.last_w.get(b)
            if w is not None:
                deps.append(w)
        for b in writes:
            w = self.last_w.get(b)
            if w is not None:
                deps.append(w)
            deps.extend(self.readers.get(b, {}).values())
        o = Op(eng, fn, deps, dma)
        o.idx = len(self.ops[eng])
        self.ops[eng].append(o)
        for b in reads:
            self.readers.setdefault(b, {})[eng if not dma else (eng, len(self.ops[eng]))] = o
        for b in writes:
            self.last_w[b] = o
            self.readers[b] = {}
        return o

    def setup(self, nc, es):
        self.esem = {e: es.enter_context(nc.semaphore("s_" + e)) for e in ENGS}
        self.dsem = [es.enter_context(nc.semaphore("d_%d" % i)) for i in range(self.n_dma_sems)]
        self.sig_base = {e: 0 for e in ENGS}
        self.dma_cnt = [0] * self.n_dma_sems
        self.dma_rr = {e: 0 for e in ENGS}
        self.prev_final = None

    def emit(self, nc):
        needs_sig = set()
        for e in ENGS:
            lastc = None
            for o in self.ops[e]:
                if not o.is_dma:
                    lastc = o
                for d in o.deps:
                    if d.is_dma or d is o:
                        continue
                    if d.eng != o.eng or d.eng != "pe":
                        needs_sig.add(id(d))
            if lastc is not None:
                needs_sig.add(id(lastc))
        for e in ENGS:
            c = self.sig_base[e]
            for o in self.ops[e]:
                if not o.is_dma and id(o) in needs_sig:
                    c += 1
                    o.sig_idx = c
            self.sig_base[e] = c
        half = self.n_dma_sems // 2
        for e in ENGS:
            base = half if e == "pool" else 0
            for o in self.ops[e]:
                if o.is_dma:
                    assert e in ("sp", "pool")
                    k = base + self.dma_rr[e] % half
                    self.dma_rr[e] += 1
                    self.dma_cnt[k] += 16
                    o.dma_sem = k
                    o.dma_val = self.dma_cnt[k]
        esem, dsem = self.esem, self.dsem
        ops = self.ops
        prev_final = self.prev_final
        with nc.Block() as block:

            def run(e, eng):
                waited = {}
                if prev_final is not None:
                    for key, val in prev_final.items():
                        if val > 0 and not (key[0] == "e" and key[1] == e):
                            s_ = dsem[key[1]] if key[0] == "d" else esem[key[1]]
                            eng.wait_ge(s_, val)
                        waited[key] = val
                for o in ops[e]:
                    w = {}
                    for d in o.deps:
                        if d is o:
                            continue
                        if d.is_dma:
                            key = ("d", d.dma_sem)
                            val = d.dma_val
                        else:
                            if d.eng == o.eng and d.eng == "pe":
                                continue
                            key = ("e", d.eng)
                            val = d.sig_idx
                        if val is None or waited.get(key, 0) >= val:
                            continue
                        if w.get(key, 0) < val:
                            w[key] = val
                    if o.is_dma and o.dma_val > 16:
                        key = ("d", o.dma_sem)
                        if waited.get(key, 0) < o.dma_val - 16 and w.get(key, 0) < o.dma_val - 16:
                            w[key] = o.dma_val - 16
                    for key, val in w.items():
                        waited[key] = val
                        s_ = dsem[key[1]] if key[0] == "d" else esem[key[1]]
                        eng.wait_ge(s_, val)
                    ins = o.fn(eng)
                    if o.is_dma:
                        ins.then_inc(dsem[o.dma_sem], 16)
                    elif o.sig_idx is not None:
                        ins.then_inc(esem[e], 1)
                fin = {}
                for o in ops[e]:
                    if o.is_dma:
                        fin[o.dma_sem] = max(fin.get(o.dma_sem, 0), o.dma_val)
                for k, v in fin.items():
                    eng.wait_ge(dsem[k], v)

            @block.tensor
            def _(eng):
                run("pe", eng)

            @block.scalar
            def _(eng):
                run("act", eng)

            @block.vector
            def _(eng):
                run("dve", eng)

            @block.gpsimd
            def _(eng):
                run("pool", eng)

            @block.sync
            def _(eng):
                run("sp", eng)
        fin = {("e", e): self.sig_base[e] for e in ENGS}
        for k in range(self.n_dma_sems):
            fin[("d", k)] = self.dma_cnt[k]
        self.prev_final = fin
        self.ops = {e: [] for e in ENGS}
        self.last_w = {}
        self.readers = {}


def build(NT, KEEP):
    NTT = NT + 1
    ROWS = NTT * 128
    nc = bass.Bass("TRN2", target_bir_lowering=False)
    R = Rec()
    esg = ExitStack()
    R.setup(nc, esg)
    es = ExitStack()

    def din(name, shape, dt=F32):
        return nc.dram_tensor(name, list(shape), dt, kind="ExternalInput").ap()

    def dout(name, shape, dt=F32):
        return nc.dram_tensor(name, list(shape), dt, kind="ExternalOutput").ap()

    def sb(name, shape, dt=F32):
        return es.enter_context(nc.sbuf_tensor(name, list(shape), dt))

    x_d = din("x", [ROWS, D])
    w_in_d = din("w_in", [D, IN_W])
    g_attn_d = din("g_attn", [1, D])
    cos_d = din("cos_t", [ROWS, 8])
    sin_d = din("sin_t", [ROWS, 8])
    ident_f_d = din("ident_f", [128, 128])
    ident_b_d = din("ident_b", [128, 128], BF)

    w_out_d = din("w_out", [D, D])
    g_ffn_d = din("g_ffn", [1, D])
    g_fin_d = din("g_fin", [1, D])
    w_rt_d = din("w_rt", [D, 36])
    b_rt_d = din("b_rt", [1, 36])
    weg_d = din("w_eg", [32, D, 512])
    weu_d = din("w_eu", [32, D, 512])
    wed_d = din("w_ed", [32, 512, D])
    wpg_d = din("w_pg", [D, D])
    wpp_d = din("w_pp", [256, D])
    p_d = din("p_all", [ROWS, 256])
    y_d = dout("y", [ROWS, D])
    wg_bf = nc.dram_tensor("wg_bf", [32 * 128, 4096], BF, kind="Internal").ap()
    wu_bf = nc.dram_tensor("wu_bf", [32 * 128, 4096], BF, kind="Internal").ap()
    wd_bf = nc.dram_tensor("wd_bf", [32 * 128, 4096], BF, kind="Internal").ap()
    sconv_d = din("state_conv", [16, 3, 1536])
    sdelta_d = din("state_delta", [16, 8, 64, 64])
    ck_d = din("cache_k", [16, 2048, 8, 64])
    cv_d = din("cache_v", [16, 2048, 8, 64])
    wconv_flat_d = din("wconv_flat", [1, 4 * 1536])
    alog128_d = din("alog128", [128, 1])
    dtb128_d = din("dtb128", [128, 1])
    cv_s_o = dout("cv_s", [16, 3, 1536])
    dl_s_o = dout("dl_s", [16, 8, 64, 64])
    pre_s_d = nc.dram_tensor("pre_s_scr", [16, 1536], F32, kind="Internal").ap()
    c3_d = nc.dram_tensor("c3_scr", [3, 16, 512], F32, kind="Internal").ap()
    z_s_d = nc.dram_tensor("z_s_scr", [16, 512], F32, kind="Internal").ap()
    qk_s_d = nc.dram_tensor("qk_s_scr", [2, 16, 512], F32, kind="Internal").ap()
    v_s_d = nc.dram_tensor("v_s_scr", [16, 512], F32, kind="Internal").ap()
    ab_s_d = nc.dram_tensor("ab_s_scr", [2, 16, 8], F32, kind="Internal").ap()
    oab_d = nc.dram_tensor("oab_scr", [2, 16, 512], F32, kind="Internal").ap()
    wk_o = dout("wk_p", [KEEP, 512])
    wv_o = dout("wv_p", [KEEP, 512])
    cv_o = dout("cv_p", [3, 1536])
    wk_s_o = dout("wk_s", [16, 512])
    wv_s_o = dout("wv_s", [16, 512])

    w_in = sb("w_in_sb", [128, 8, IN_W], BF)
    gB = sb("gB", [128, D])
    ident_f = sb("ident_f_sb", [128, 128])
    ident_b = sb("ident_b_sb", [128, 128], BF)
    xt = [sb("xt%d" % i, [128, D]) for i in range(2)]
    ss = sb("ss", [128, 1])
    rstd = sb("rstd", [128, 1])
    xn = sb("xn", [128, D], BF)
    xnT = sb("xnT", [128, 8, 128], BF)
    cs_t = [sb("cs_t%d" % i, [128, 16]) for i in range(2)]
    qk_sb = sb("qk_sb", [128, 16, 64])
    v_sb = sb("v_sb", [128, 512])

    pw = esg.enter_context(nc.psum_tensor("pw", [128, 1536], F32))
    pt = esg.enter_context(nc.psum_tensor("pt", [128, 512], F32))
    ps2 = esg.enter_context(nc.psum_tensor("ps2", [128, 1024], F32))
    po2 = esg.enter_context(nc.psum_tensor("po2", [128, 1024], F32))
    banks = [pw[:, 0:512], pw[:, 512:1024], pw[:, 1024:1536], pt[:, :], ps2[:, 0:512], ps2[:, 512:1024],
             po2[:, 0:512], po2[:, 512:1024]]
    kT_ring = sb("kT_ring", [128, 4, NWIN * 128], BF)
    V_ring = sb("V_ring", [128, NWIN, 8, 65], BF)
    qTe = sb("qTe", [128, 4, 128], BF)
    qTo = sb("qTo", [128, 4, 128], BF)
    qkb = sb("qkb", [128, 1024], BF)
    pexp = [sb("pexp%d" % i, [128, 8, 128], BF) for i in range(2)]
    pm = [sb("pm%d" % i, [128, 8, 128], BF) for i in range(2)]
    Cm = sb("Cm", [128, NWIN, 128], BF)
    rden = sb("rden", [128, 8])
    ssb = sb("ssb", [128, 8])
    gbB = sb("gbB", [128, 64])
    o_cat = sb("o_cat", [128, 1024], BF)
    wconv_d = din("wconvT", [128, 48])
    alog_d = din("a_log", [1, 8])
    dtb_d = din("dt_bias", [1, 8])
    g_a_d = din("g_a", [1, 64])
    tri_d = din("triF", [128, 128])
    ones_d = din("onesF", [128, 128])
    tris_d = din("triS", [128, 128])
    pidx_d = din("pidx", [128, 1])
    b128_d = din("b128", [1, (2 * ROWS) // 256 + 32])
    trin_d = din("triN", [128, 128])
    mUi_d = din("maskUi", [128, 128])
    mUs_d = din("maskUs", [128, 128])
    mLs_d = din("maskLs", [128, 128])
    dl_o = dout("dl_p", [128, 256])
    ocat_d = nc.dram_tensor("ocat_scr", [ROWS, 1024], BF, kind="Internal").ap()
    wT = sb("wT", [128, 12, 4])
    nexpA = sb("nexpA", [128, 8])
    dtbB = sb("dtbB", [128, 8])
    gaB = sb("gaB", [128, 64])
    triF = sb("triF_sb", [128, 128])
    onesF = sb("onesF_sb", [128, 128])
    mUi = sb("mUi", [128, 128])
    mUs = sb("mUs", [128, 128])
    mLs = sb("mLs", [128, 128])
    xc = sb("xc", [128, 12, 131])
    ca = sb("ca", [128, 1536])
    cb = sb("cb", [128, 1536])
    pre_sb = ca
    sq = cb[:, 0:1024]
    rn = sb("rn", [128, 16])
    zs = sb("zs", [128, 512])
    sm = sb("sm", [128, 80])
    kp_f = sb("kp_f", [128, 8, 64])
    kpqs_b = sb("kpqs_b", [128, 1024], BF)
    kgqg_b = sb("kgqg_b", [128, 1024], BF)
    kd_b = sb("kd_b", [128, 512], BF)
    vb_f = sb("vb_f", [128, 512])
    r_f = vb_f
    kpT = sb("kpT", [128, 4, 128], BF)
    kpTe = sb("kpTe", [128, 4, 128], BF)
    kpTo = sb("kpTo", [128, 4, 128], BF)
    qsTe = sb("qsTe", [128, 4, 128], BF)
    qsTo = sb("qsTo", [128, 4, 128], BF)
    kgqgT = sb("kgqgT", [128, 8, 128], BF)
    LaB = sb("LaB", [128, 8, 128])
    dtmp = LaB
    triN = sb("triN_sb", [128, 128])
    decUi = sb("decUi", [128, 8, 128], BF)
    Am = [sb("Am%d" % i, [128, 8, 128]) for i in range(2)]
    Bm = [sb("Bm%d" % i, [128, 8, 128]) for i in range(2)]
    Rm = sb("Rm", [128, 8, 128])
    MTs = sb("MTs", [128, 8, 128], BF)
    S_sb = sb("S_sb", [128, 4, 64])
    S_bd = sb("S_bd", [128, 4, 128], BF)
    x_b = sb("x_b", [128, 512], BF)
    o_f = sb("o_f", [128, 8, 64])
    ob_f = o_f
    rt = [o_f[:, 2 * i:2 * i + 2, :].rearrange("p a (h d) -> p (a h) d", d=8) for i in range(4)]
    cm_d = din("cmask", [128, NWIN * 128], BF)
    g_b_d = din("g_b", [1, 64])

    def dma(out, in_, reads=(), writes=(), eng="sp"):
        return R.op(eng, lambda e: e.dma_start(out=out, in_=in_), reads, writes, dma=True)

    def act(out, in_, func, reads=(), writes=(), **kw):
        return R.op("act", lambda e: e.activation(out=out, in_=in_, func=func, **kw), reads, writes)

    def mm(out, lhsT, rhs, start, stop, reads=(), writes=(), sgc=False):
        return R.op("pe", lambda e: e.matmul(out, lhsT, rhs, start=start, stop=stop, skip_group_check=sgc),
                    reads, writes)

    def tr(out, in_, ident, reads=(), writes=()):
        return R.op("pe", lambda e: e.transpose(out, in_, ident), reads, writes)

    def tt(eng, out, in0, in1, op, reads=(), writes=()):
        return R.op(eng, lambda e: e.tensor_tensor(out, in0, in1, op), reads, writes)

    def ts(eng, out, in0, s1, s2, op0, op1=None, reads=(), writes=()):
        if op1 is None:
            return R.op(eng, lambda e: e.tensor_scalar(out, in0, s1, None, op0), reads, writes)
        return R.op(eng, lambda e: e.tensor_scalar(out, in0, s1, s2, op0, op1), reads, writes)

    def stt(out, in0, scalar, in1, op0, op1, reads=(), writes=()):
        return R.op("dve", lambda e: e.scalar_tensor_tensor(out, in0, scalar, in1, op0, op1), reads, writes)

    def cp(eng, out, in_, reads=(), writes=()):
        if eng == "act":
            return R.op("act", lambda e: e.copy(out, in_), reads, writes)
        return R.op(eng, lambda e: e.tensor_copy(out, in_), reads, writes)

    def red(out, in_, op, reads=(), writes=()):
        return R.op("dve", lambda e: e.tensor_reduce(out, in_, AX.X, op), reads, writes)

    def recip(out, in_, reads=(), writes=()):
        return R.op("dve", lambda e: e.reciprocal(out, in_), reads, writes)

    dma(ident_f[:], ident_f_d, writes=["ident_f"])
    dma(ident_b[:], ident_b_d, writes=["ident_b"])
    dma(gB[:], g_attn_d.partition_broadcast(128), writes=["gB"])
    w_in_v = w_in_d.rearrange("(k p) n -> p k n", p=128)
    for k in range(8):
        dma(w_in[:, k, :], w_in_v[:, k, :], writes=["w_in"], eng="pool")

    for e_ in range(32 if not os.environ.get('NOCONV') else 0):
        dma(wg_bf[e_ * 128:(e_ + 1) * 128, :].rearrange("p (k n) -> p k n", k=8),
            weg_d[e_].rearrange("(k p) n -> p k n", p=128), eng="pool")
        dma(wu_bf[e_ * 128:(e_ + 1) * 128, :].rearrange("p (k n) -> p k n", k=8),
            weu_d[e_].rearrange("(k p) n -> p k n", p=128), eng="pool")
        dma(wd_bf[e_ * 128:(e_ + 1) * 128, :].rearrange("p (c n) -> p c n", c=4),
            wed_d[e_].rearrange("(c p) n -> p c n", p=128), eng="pool")

    def rmsnorm_T(t):
        s = t % 2
        dma(xt[s][:], x_d[t * 128:(t + 1) * 128, :], writes=["xt%d" % s])
        dma(cs_t[s][:, 0:8], cos_d[t * 128:(t + 1) * 128, :], writes=["cs%d" % s])
        dma(cs_t[s][:, 8:16], sin_d[t * 128:(t + 1) * 128, :], writes=["cs%d" % s])
        act(sq[:], xt[s][:], AF.Square, reads=["xt%d" % s], writes=["cb"])
        red(ss[:], sq[:], ALU.add, reads=["cb"], writes=["ss"])
        ts("dve", ss[:], ss[:], 1.0 / D, EPS, ALU.mult, ALU.add, reads=["ss"], writes=["ss"])
        act(ss[:], ss[:], AF.Sqrt, reads=["ss"], writes=["ss"])
        recip(rstd[:], ss[:], reads=["ss"], writes=["rstd"])
        stt(xn[:], xt[s][:], rstd[:], gB[:], ALU.mult, ALU.mult,
            reads=["xt%d" % s, "rstd", "gB"], writes=["xn"])
        pT = banks[3][:].bitcast(BF)
        for k in range(8):
            tr(pT[:, k * 128:(k + 1) * 128], xn[:, k * 128:(k + 1) * 128], ident_b[:],
               reads=["xn", "ident_b"], writes=["b3"])
        cp("act", xnT[:].rearrange("p k t -> p (k t)"), pT, reads=["b3"], writes=["xnT"])

    def inproj_tm(col0, ncols, bank_ids, bname):
        done = 0
        bi = 0
        while done < ncols:
            n = min(512, ncols - done)
            for k in range(8):
                mm(banks[bank_ids[bi]][:, 0:n], xnT[:, k, :], w_in[:, k, col0 + done:col0 + done + n],
                   start=(k == 0), stop=(k == 7), reads=["xnT", "w_in"], writes=[bname[bi]])
            done += n
            bi += 1

    def window_qkv(t):
        s = t % 2
        inproj_tm(OFF_WIN, 1536, [0, 1, 2], ["b0", "b1", "b2"])
        for j, b in enumerate((0, 1)):
            pv = banks[b][:].rearrange("p (h d) -> p h d", d=64)
            qs = qk_sb[:, j * 8:(j + 1) * 8, :]
            cosB = cs_t[s][:, None, 0:8].to_broadcast([128, 8, 8])
            sinB = cs_t[s][:, None, 8:16].to_broadcast([128, 8, 8])
            x1 = pv[:, :, 0:8]
            x2 = pv[:, :, 8:16]
            r0 = rt[0][:, j * 8:(j + 1) * 8, :]
            r1 = rt[1][:, j * 8:(j + 1) * 8, :]
            r2 = rt[2][:, j * 8:(j + 1) * 8, :]
            r3 = rt[3][:, j * 8:(j + 1) * 8, :]
            bn = "b%d" % b
            tt("dve", r0, x1, cosB, ALU.mult, reads=[bn, "cs%d" % s], writes=["o_f"])
            tt("dve", r1, x2, sinB, ALU.mult, reads=[bn, "cs%d" % s], writes=["o_f"])
            tt("dve", r2, x2, cosB, ALU.mult, reads=[bn, "cs%d" % s], writes=["o_f"])
            tt("dve", r3, x1, sinB, ALU.mult, reads=[bn, "cs%d" % s], writes=["o_f"])
            tt("dve", qs[:, :, 0:8], r0, r1, ALU.subtract, reads=["o_f"], writes=["qk_sb"])
            tt("dve", qs[:, :, 8:16], r2, r3, ALU.add, reads=["o_f"], writes=["qk_sb"])
            cp("act", qs[:, :, 16:64], pv[:, :, 16:64], reads=[bn], writes=["qk_sb"])
        cp("act", v_sb[:], banks[2][:], reads=["b2"], writes=["v_sb"])

    def outputs_kv(t):
        if t < NT:
            first = NT - KEEP // 128
            if t >= first:
                r0 = (t - first) * 128
                dma(wk_o[r0:r0 + 128, :], qk_sb[:, 8:16, :].rearrange("p h d -> p (h d)"), reads=["qk_sb"])
                dma(wv_o[r0:r0 + 128, :], v_sb[:], reads=["v_sb"])
        else:
            dma(wk_s_o[:, :], qk_sb[0:16, 8:16, :].rearrange("p h d -> p (h d)"), reads=["qk_sb"])
            dma(wv_s_o[:, :], v_sb[0:16, :], reads=["v_sb"])

    def preconv_tm(t):
        inproj_tm(0, 1536, [0, 1, 2], ["b0", "b1", "b2"])
        for j, b in enumerate((0, 1, 2)):
            cp("act", pre_sb[:, j * 512:(j + 1) * 512], banks[b][:], reads=["b%d" % b], writes=["ca"])
        if t == NT - 1:
            dma(cv_o[:, :], pre_sb[125:128, :], reads=["ca"])
        if t == NT:
            dma(pre_s_d[:, :], pre_sb[0:16, :], reads=["ca"])
            for k in range(8):
                mm(ps2[:, 0:512], xnT[:, k, :], w_in[:, k, OFF_Z:OFF_Z + 512], k == 0, k == 7, reads=["xnT", "w_in"],
                   writes=["ps2"])
            for k in range(8):
                mm(ps2[:, 512:528], xnT[:, k, :], w_in[:, k, OFF_A:OFF_A + 16], k == 0, k == 7, reads=["xnT", "w_in"],
                   writes=["ps2"])
            cp("act", zs[:], ps2[:, 0:512], reads=["ps2"], writes=["zs"])
            cp("act", sm[:, 0:16], ps2[:, 512:528], reads=["ps2"], writes=["sm_xa"])
            dma(z_s_d[:, :], zs[0:16, :], reads=["zs"])
            dma(ab_s_d[0], sm[0:16, 0:8], reads=["sm_xa"])
            dma(ab_s_d[1], sm[0:16, 8:16], reads=["sm_xa"])
            dma(qk_s_d[0], qk_sb[0:16, 0:8, :].rearrange("p h d -> p (h d)"), reads=["qk_sb"])
            dma(qk_s_d[1], qk_sb[0:16, 8:16, :].rearrange("p h d -> p (h d)"), reads=["qk_sb"])
            dma(v_s_d[:, :], v_sb[0:16, :], reads=["v_sb"])

    dma(Cm[:].rearrange("p a b -> p (a b)"), cm_d, writes=["Cm"])
    dma(gbB[:], g_b_d.partition_broadcast(128), writes=["gbB"])
    R.op("pool", lambda e: e.memset(V_ring[:].rearrange("p a h d -> p (a h d)"), 1.0), writes=["V_ring"])
    R.op("pool", lambda e: e.memset(qTe[:].rearrange("p a t -> p (a t)"), 0.0), writes=["qT"])
    R.op("pool", lambda e: e.memset(qTo[:].rearrange("p a t -> p (a t)"), 0.0), writes=["qT"])

    def attn_finish(np_, tag):
        pov = po2[0:np_, :].rearrange("p (h d) -> p h d", d=128)
        R.op("dve", lambda e: e.reciprocal(rden[0:np_, :], pov[:, :, 64]), ["po2"], ["rden"])
        tt("dve", ob_f[0:np_], pov[:, :, 0:64], rden[0:np_, :, None].to_broadcast([np_, 8, 64]), ALU.mult,
           reads=["po2", "rden"], writes=["o_f"])
        act(sq[0:np_, 0:512], ob_f[0:np_].rearrange("p h d -> p (h d)"), AF.Square, reads=["o_f"], writes=["cb"])
        red(ssb[0:np_, :], sq[0:np_, 0:512].rearrange("p (h d) -> p h d", d=64), ALU.add, reads=["cb"], writes=["ssb"])
        ts("dve", ssb[0:np_, :], ssb[0:np_, :], 1.0 / 64, EPS, ALU.mult, ALU.add, reads=["ssb"], writes=["ssb"])
        act(ssb[0:np_, :], ssb[0:np_, :], AF.Sqrt, reads=["ssb"], writes=["ssb"])
        recip(ssb[0:np_, :], ssb[0:np_, :], reads=["ssb"], writes=["ssb"])
        tt("dve", ob_f[0:np_], ob_f[0:np_], ssb[0:np_, :, None].to_broadcast([np_, 8, 64]), ALU.mult,
           reads=["o_f", "ssb"], writes=["o_f"])
        tt("dve", o_cat[0:np_, 512:1024].rearrange("p (h d) -> p h d", d=64), ob_f[0:np_],
           gbB[0:np_, None, :].to_broadcast([np_, 8, 64]), ALU.mult, reads=["o_f", "gbB"], writes=["o_cat"])

    def attn_prompt(t):
        sl = t % NWIN
        cp("act", qkb[:], qk_sb[:].rearrange("p h d -> p (h d)"), reads=["qk_sb"], writes=["qkb"])
        cp("pool", V_ring[:, sl, :, 0:64], v_sb[:].rearrange("p (h d) -> p h d", d=64),
           reads=["v_sb"], writes=["V_ring"])
        pT = pt[:, :].bitcast(BF)
        for c in range(8):
            tr(pT[:, c * 128:(c + 1) * 128], qkb[:, c * 128:(c + 1) * 128], ident_b[:],
               reads=["qkb", "ident_b"], writes=["b3"])
        cp("act", qTe[0:64].rearrange("p a t -> p (a t)"), pT[0:64, 0:512], reads=["b3"], writes=["qT"])
        cp("act", qTo[64:128].rearrange("p a t -> p (a t)"), pT[64:128, 0:512], reads=["b3"], writes=["qT"])
        cp("act", kT_ring[:, :, sl * 128:(sl + 1) * 128], pT[:, 512:1024].rearrange("p (a t) -> p a t", t=128),
           reads=["b3"], writes=["kT_ring"])
        LV = int(os.environ.get('ATT_LEVEL', '9'))
        nd = min(t, 16) + 1
        nst = 2 * nd

        def scores(st):
            dl, hg = st // 2, st % 2
            s2 = (t - dl) % NWIN
            buf = ps2[:, (st % 2) * 512:(st % 2 + 1) * 512]
            bn = ["ps2a" if st % 2 == 0 else "ps2b"]
            for hh in range(4):
                h = 4 * hg + hh
                hp, pr = h % 2, h // 2
                mm(buf[:, hh * 128:(hh + 1) * 128], kT_ring[:, pr, s2 * 128:(s2 + 1) * 128],
                   (qTo if hp else qTe)[:, pr, :], True, True, reads=["kT_ring", "qT"], writes=bn)
            pe_ = pexp[st % 2][:, 0:4, :]
            act(pe_.rearrange("p h t -> p (h t)"), buf, AF.Exp, reads=bn, writes=["pexp%d" % (st % 2)], scale=0.125)
            tt("pool" if st % 2 == 0 else "dve", pm[st % 2][:, 0:4, :], pe_,
               Cm[:, dl:dl + 1, :].to_broadcast([128, 4, 128]), ALU.mult,
               reads=["pexp%d" % (st % 2), "Cm"], writes=["pm%d" % (st % 2)])

        def pv(st):
            dl, hg = st // 2, st % 2
            s2 = (t - dl) % NWIN
            pmb = pm[st % 2]
            for hh in range(4):
                h = 4 * hg + hh
                mm(po2[:, h * 128:h * 128 + 65], pmb[:, hh, :], V_ring[:, s2, h, :], dl == 0 and hh == 0, dl == nd - 1,
                   reads=["pm%d" % (st % 2), "V_ring"], writes=["po2a" if hg == 0 else "po2b"], sgc=True)

        def gen():
            scores(0)
            yield
            for st in range(nst):
                if st + 1 < nst:
                    scores(st + 1)
                pv(st)
                yield
        return gen()

    def attn_done(t):
        LV = 9
        if LV < 5:
            return
        attn_finish(128, "p")


    for nm, tl, dd in (("triN", triN, trin_d), ("triF", triF, tri_d), ("onesF", onesF, ones_d), ("mUi", mUi, mUi_d), ("mUs", mUs, mUs_d),
                       ("mLs", mLs, mLs_d)):
        dma(tl[:], dd, writes=[nm])
    dma(wT[:].rearrange("p c j -> p (c j)"), wconv_d, writes=["wT"])
    dma(nexpA[:], alog_d.partition_broadcast(128), writes=["nexpA"])
    dma(dtbB[:], dtb_d.partition_broadcast(128), writes=["dtbB"])
    dma(gaB[:], g_a_d.partition_broadcast(128), writes=["gaB"])
    act(nexpA[:], nexpA[:], AF.Exp, reads=["nexpA"], writes=["nexpA"])
    ts("dve", nexpA[:], nexpA[:], -1.0, None, ALU.mult, reads=["nexpA"], writes=["nexpA"])
    R.op("pool", lambda e: e.memset(xc[:].rearrange("p c t -> p (c t)"), 0.0), writes=["xc"])
    R.op("pool", lambda e: e.memset(S_sb[:].rearrange("p a d -> p (a d)"), 0.0), writes=["S_sb"])
    R.op("pool", lambda e: e.memset(S_bd[:].rearrange("p a d -> p (a d)"), 0.0), writes=["S_bd"])
    for tl, nm in ((kpTe, "kpTe"), (kpTo, "kpTo"), (qsTe, "qsTe"), (qsTo, "qsTo")):
        R.op("pool", (lambda tl: (lambda e: e.memset(tl[:].rearrange("p a t -> p (a t)"), 0.0)))(tl), writes=[nm])

    def bc8(ap, n=64):
        return ap[:, :, None].to_broadcast([128, 8, n])

    GLV = int(os.environ.get('GDN_LEVEL', '9'))
    G4 = int(os.environ.get('G4', '99'))
    G7 = int(os.environ.get('G7', '99'))

    def gdn_front(t):
        for c in range(12):
            for k in range(8):
                mm(pw[:, c * 128:(c + 1) * 128], w_in[:, k, c * 128:(c + 1) * 128], xnT[:, k, :], k == 0, k == 7,
                   reads=["xnT", "w_in"], writes=["b0", "b1", "b2"])
        if t > 0:
            cp("pool", xc[:, :, 0:3], xc[:, :, 128:131], reads=["xc"], writes=["xc"])
        cp("act", xc[:, :, 3:131], pw[:, :].rearrange("p (c t) -> p c t", t=128), reads=["b0", "b1", "b2"], writes=["xc"])
        ca3 = ca[:].rearrange("p (c t) -> p c t", t=128)
        cb3 = cb[:].rearrange("p (c t) -> p c t", t=128)

        def wb(j):
            return wT[:, :, j:j + 1].to_broadcast([128, 12, 128])
        for eng_, lo_, hi_, sfx in (("pool", 0, 5, "_l"), ("dve", 5, 12, "_h")):
            cah, cbh = ca3[:, lo_:hi_, :], cb3[:, lo_:hi_, :]

            def wbh(j, lo_=lo_, hi_=hi_):
                return wT[:, lo_:hi_, j:j + 1].to_broadcast([128, hi_ - lo_, 128])
            tt(eng_, cah, xc[:, lo_:hi_, 3:131], wbh(3), ALU.mult, reads=["xc", "wT"], writes=["ca" + sfx])
            for j in (2, 1, 0):
                tt(eng_, cbh, xc[:, lo_:hi_, j:j + 128], wbh(j), ALU.mult, reads=["xc", "wT"], writes=["cb" + sfx])
                tt(eng_, cah, cah, cbh, ALU.add, reads=["ca" + sfx, "cb" + sfx], writes=["ca" + sfx])
        act(cb[:], ca[:], AF.Silu, reads=["ca"], writes=["cb"])

    def gdn_prompt(t):
        ca3 = ca[:].rearrange("p (c t) -> p c t", t=128)
        if GLV <= 1:
            return
        for c in range(12):
            tr(pw[:, c * 128:(c + 1) * 128], cb[:, c * 128:(c + 1) * 128], ident_f[:], reads=["cb", "ident_f"],
               writes=["b0", "b1", "b2"])
        cp("act", ca[:], pw[:, :], reads=["b0", "b1", "b2"], writes=["ca"])
        ca_qk = ca[:, 0:1024].rearrange("p (h d) -> p h d", d=64)
        ca_q = ca[:, 0:512].rearrange("p (h d) -> p h d", d=64)
        ca_k = ca[:, 512:1024].rearrange("p (h d) -> p h d", d=64)
        ca_v = ca[:, 1024:1536].rearrange("p (h d) -> p h d", d=64)
        act(sq[:], ca[:, 0:1024], AF.Square, reads=["ca"], writes=["cb"])
        red(rn[:], sq[:].rearrange("p (h d) -> p h d", d=64), ALU.add, reads=["cb"], writes=["rn"])
        ts("dve", rn[:], rn[:], EPS, None, ALU.add, reads=["rn"], writes=["rn"])
        act(rn[:], rn[:], AF.Sqrt, reads=["rn"], writes=["rn"])
        recip(rn[:], rn[:], reads=["rn"], writes=["rn"])
        tt("dve", ca_qk, ca_qk, rn[:, :, None].to_broadcast([128, 16, 64]), ALU.mult, reads=["ca", "rn"], writes=["ca"])
        if GLV <= 2:
            return
        for k in range(8):
            mm(ps2[:, 0:512], xnT[:, k, :], w_in[:, k, OFF_Z:OFF_Z + 512], k == 0, k == 7, reads=["xnT", "w_in"],
               writes=["ps2"])
        for k in range(8):
            mm(ps2[:, 512:528], xnT[:, k, :], w_in[:, k, OFF_A:OFF_A + 16], k == 0, k == 7, reads=["xnT", "w_in"],
               writes=["ps2"])
        act(zs[:], ps2[:, 0:512], AF.Silu, reads=["ps2"], writes=["zs"])
        xa, la, beta, sbt = sm[:, 0:8], sm[:, 8:16], sm[:, 16:24], sm[:, 24:32]
        g_, eg, egl, eglg, dgl, sso = sm[:, 32:40], sm[:, 40:48], sm[:, 48:56], sm[:, 56:64], sm[:, 64:72], sm[:, 72:80]
        tt("dve", xa, ps2[:, 512:520], dtbB[:], ALU.add, reads=["ps2", "dtbB"], writes=["sm_xa"])
        act(beta, ps2[:, 520:528], AF.Sigmoid, reads=["ps2"], writes=["sm_beta"])
        act(sbt, beta, AF.Sqrt, reads=["sm_beta"], writes=["sm_sbt"])
        act(xa, xa, AF.Exp, reads=["sm_xa"], writes=["sm_xa"])
        act(xa, xa, AF.Ln, reads=["sm_xa"], writes=["sm_xa"], bias=1.0)
        tt("dve", la, xa, nexpA[:], ALU.mult, reads=["sm_xa", "nexpA"], writes=["sm_la"])
        mm(pt[:, 0:8], triF[:], la, True, True, reads=["triF", "sm_la"], writes=["b3"])
        mm(pt[:, 8:16], onesF[:], la, True, True, reads=["onesF", "sm_la"], writes=["b3"])
        cp("dve", g_, pt[:, 0:8], reads=["b3"], writes=["sm_g"])
        act(eg, pt[:, 0:8], AF.Exp, reads=["b3"], writes=["sm_eg"])
        act(egl, pt[:, 8:16], AF.Exp, reads=["b3"], writes=["sm_egl"])
        tt("dve", dgl, pt[:, 8:16], g_, ALU.subtract, reads=["b3", "sm_g"], writes=["sm_dgl"])
        act(eglg, dgl, AF.Exp, reads=["sm_dgl"], writes=["sm_eglg"])
        if GLV <= 3:
            return
        if G4 < 1:
            return
        tt("dve", kp_f[:], ca_k, bc8(sbt), ALU.mult, reads=["ca", "sm_sbt"], writes=["kp_f"])
        if G4 < 2:
            return
        cp("act", kpqs_b[:, 0:512], kp_f[:].rearrange("p h d -> p (h d)"), reads=["kp_f"], writes=["kpqs_b"])
        if G4 < 3:
            return
        act(kpqs_b[:, 512:1024], ca[:, 0:512], AF.Copy, reads=["ca"], writes=["kpqs_b"], scale=0.125)
        if G4 < 4:
            return
        tt("pool", kgqg_b[:, 0:512].rearrange("p (h d) -> p h d", d=64), kp_f[:], bc8(eg), ALU.mult,
           reads=["kp_f", "sm_eg"], writes=["kgqg_b"])
        if G4 < 5:
            return
        stt(kgqg_b[:, 512:1024].rearrange("p (h d) -> p h d", d=64), ca_q, 0.125, bc8(eg), ALU.mult, ALU.mult,
            reads=["ca", "sm_eg"], writes=["kgqg_b"])
        if G4 < 6:
            return
        tt("pool", kd_b[:].rearrange("p (h d) -> p h d", d=64), kp_f[:], bc8(eglg), ALU.mult,
           reads=["kp_f", "sm_eglg"], writes=["kd_b"])
        if G4 < 7:
            return
        tt("dve", vb_f[:].rearrange("p (h d) -> p h d", d=64), ca_v, bc8(sbt), ALU.mult, reads=["ca", "sm_sbt"],
           writes=["vb_f"])
        if G4 < 8:
            return
        ps2b = ps2[:, :].bitcast(BF)
        if G4 < 9:
            return
        for i in range(8):
            tr(ps2b[:, i * 128:(i + 1) * 128], kpqs_b[:, i * 128:(i + 1) * 128], ident_b[:],
               reads=["kpqs_b", "ident_b"], writes=["ps2"])
        if G4 < 10:
            return
        for i in range(8):
            tr(ps2b[:, 1024 + i * 128:1024 + (i + 1) * 128], kgqg_b[:, i * 128:(i + 1) * 128], ident_b[:],
               reads=["kgqg_b", "ident_b"], writes=["ps2"])
        if G4 < 11:
            return
        cp("act", kpT[:].rearrange("p a t -> p (a t)"), ps2b[:, 0:512], reads=["ps2"], writes=["kpT"])
        if G4 < 12:
            return
        cp("act", kpTe[0:64].rearrange("p a t -> p (a t)"), ps2b[0:64, 0:512], reads=["ps2"], writes=["kpTe"])
        if G4 < 13:
            return
        cp("act", kpTo[64:128].rearrange("p a t -> p (a t)"), ps2b[64:128, 0:512], reads=["ps2"], writes=["kpTo"])
        if G4 < 14:
            return
        cp("act", qsTe[0:64].rearrange("p a t -> p (a t)"), ps2b[0:64, 512:1024], reads=["ps2"], writes=["qsTe"])
        if G4 < 15:
            return
        cp("act", qsTo[64:128].rearrange("p a t -> p (a t)"), ps2b[64:128, 512:1024], reads=["ps2"], writes=["qsTo"])
        if G4 < 16:
            return
        cp("act", kgqgT[:].rearrange("p a t -> p (a t)"), ps2b[:, 1024:2048], reads=["ps2"], writes=["kgqgT"])
        if GLV <= 4:
            return
        cp("dve", LaB[:], la[:, :, None].to_broadcast([128, 8, 128]), reads=["sm_la"], writes=["LaB"])
        for h in range(8):
            mm(po2[:, h * 128:(h + 1) * 128], LaB[:, h, :], triF[:], True, False, reads=["LaB", "triF"], writes=["po2"])
            mm(po2[:, h * 128:(h + 1) * 128], triN[:], LaB[:, h, :], False, True, reads=["LaB", "triN"],
               writes=["po2"])
        po2v = po2[:, :].rearrange("p (h t) -> p h t", t=128)
        ps2v = ps2[:, :].rearrange("p (h t) -> p h t", t=128)

        def mb(m):
            return m[:, None, :].to_broadcast([128, 8, 128])
        for h in range(8):
            hp, pr = h % 2, h // 2
            mm(ps2[:, h * 128:(h + 1) * 128], kpT[:, pr, :], (kpTo if hp else kpTe)[:, pr, :], True, True,
               reads=["kpT", "kpTe", "kpTo"], writes=["ps2"])
        stt(dtmp[:], po2v, -1.0, mb(mLs), ALU.mult, ALU.add, reads=["po2", "mLs"], writes=["LaB"])
        act(dtmp[:], dtmp[:], AF.Exp, reads=["LaB"], writes=["LaB"])
        tt("dve", Am[0][:], ps2v, dtmp[:], ALU.mult, reads=["ps2", "LaB"], writes=["Am0_0", "Am0_1"])
        tt("dve", dtmp[:], po2v, mb(mUs), ALU.add, reads=["po2", "mUs"], writes=["LaB"])
        act(dtmp[:], dtmp[:], AF.Exp, reads=["LaB"], writes=["LaB"])
        tt("dve", Bm[0][:], ps2v, dtmp[:], ALU.mult, reads=["ps2", "LaB"], writes=["Bm0_0", "Bm0_1"])
        tt("dve", dtmp[:], po2v, mb(mUi), ALU.add, reads=["po2", "mUi"], writes=["LaB"])
        act(decUi[:], dtmp[:], AF.Exp, reads=["LaB"], writes=["decUi"])
        for h in range(8):
            hp, pr = h % 2, h // 2
            mm(ps2[:, h * 128:(h + 1) * 128], kpT[:, pr, :], (qsTo if hp else qsTe)[:, pr, :], True, True,
               reads=["kpT", "qsTe", "qsTo"], writes=["ps2"])
        tt("dve", MTs[:], ps2v, decUi[:], ALU.mult, reads=["ps2", "decUi"], writes=["MTs"])
        if GLV <= 5:
            return
        tt("pool", Rm[:], ident_f[:, None, :].to_broadcast([128, 8, 128]), Bm[0][:], ALU.subtract,
           reads=["ident_f", "Bm0_0", "Bm0_1"], writes=["Rm0", "Rm1"])
        def neu():
            cur = 0
            for lvl in range(6):
                last = lvl == 5
                nx = 1 - cur
                for half in range(2):
                    hs = range(4 * half, 4 * half + 4)
                    for h in hs:
                        mm(pw[:, (h % 4) * 128:(h % 4 + 1) * 128], Bm[cur][:, h, :], Am[cur][:, h, :], True, True,
                           reads=["Am%d_%d" % (cur, half), "Bm%d_%d" % (cur, half)], writes=["b0"])
                    if not last:
                        for h in hs:
                            mm(pw[:, 512 + (h % 4) * 128:512 + (h % 4 + 1) * 128], Am[cur][:, h, :], Bm[cur][:, h, :],
                               True, True, reads=["Am%d_%d" % (cur, half), "Bm%d_%d" % (cur, half)], writes=["b1"])
                    cp("act", Am[nx][:, 4 * half:4 * half + 4, :].rearrange("p h t -> p (h t)"), pw[:, 0:512],
                       reads=["b0"], writes=["Am%d_%d" % (nx, half)])
                    if not last:
                        ts("dve", Bm[nx][:, 4 * half:4 * half + 4, :].rearrange("p h t -> p (h t)"), pw[:, 512:1024],
                           1.0, None, ALU.mult, reads=["b1"], writes=["Bm%d_%d" % (nx, half)])
                    yield
                    for h in hs:
                        mm(pw[:, 1024 + (h % 4) * 128:1024 + (h % 4 + 1) * 128], Am[nx][:, h, :], Rm[:, h, :], True,
                           True, reads=["Am%d_%d" % (nx, half), "Rm%d" % half], writes=["b2"])
                    rv = Rm[:, 4 * half:4 * half + 4, :].rearrange("p h t -> p (h t)")
                    tt("dve", rv, rv, pw[:, 1024:1536], ALU.add, reads=["Rm%d" % half, "b2"], writes=["Rm%d" % half])
                    yield
                cur = nx
        return neu()

    def gdn_rec(t):
        xa, la, beta, sbt = sm[:, 0:8], sm[:, 8:16], sm[:, 16:24], sm[:, 24:32]
        g_, eg, egl, eglg, dgl, sso = sm[:, 32:40], sm[:, 40:48], sm[:, 48:56], sm[:, 56:64], sm[:, 64:72], sm[:, 72:80]
        if GLV <= 6:
            return
        kgT = kgqgT[:, 0:4, :]
        qgT = kgqgT[:, 4:8, :]
        if G7 < 1:
            return
        for pr in range(4):
            mm(pt[:, pr * 128:(pr + 1) * 128], kgT[:, pr, :], S_bd[:, pr, :], True, True, reads=["kgqgT", "S_bd"],
               writes=["b3"])
        if G7 < 2:
            return
        tt("dve", r_f[:], vb_f[:], pt[:, :], ALU.subtract, reads=["vb_f", "b3"], writes=["vb_f"])
        if G7 < 3:
            return
        for h in range(8):
            mm(pt[:, h * 64:(h + 1) * 64], Rm[:, h, :], r_f[:, h * 64:(h + 1) * 64], True, True, reads=["Rm0", "Rm1", "vb_f"],
               writes=["b3"])
        if G7 < 4:
            return
        cp("act", x_b[:], pt[:, :], reads=["b3"], writes=["x_b"])
        if G7 < 5:
            return
        for pr in range(4):
            mm(pt[:, pr * 128:(pr + 1) * 128], qgT[:, pr, :], S_bd[:, pr, :], True, False, reads=["kgqgT", "S_bd"],
               writes=["b3"], sgc=True)
            for hh in (2 * pr, 2 * pr + 1):
                mm(pt[:, hh * 64:(hh + 1) * 64], MTs[:, hh, :], x_b[:, hh * 64:(hh + 1) * 64], False, hh % 2 == 1,
                   reads=["MTs", "x_b"], writes=["b3"], sgc=True)
        if G7 < 6:
            return
        cp("act", o_f[:].rearrange("p h d -> p (h d)"), pt[:, :], reads=["b3"], writes=["o_f"])
        if G7 < 7:
            return
        for pr in range(4):
            mm(pt[:, pr * 128:(pr + 1) * 128], kd_b[:, pr * 128:(pr + 1) * 128], x_b[:, pr * 128:(pr + 1) * 128],
               True, True, reads=["kd_b", "x_b"], writes=["b3"])
        if G7 < 8:
            return
        for hp in range(2):
            rows = slice(64 * hp, 64 * hp + 64)
            eglh = sm[rows, 48 + hp:56:2]
            tt("dve", S_sb[rows], S_sb[rows], eglh[:, :, None].to_broadcast([64, 4, 64]), ALU.mult,
               reads=["S_sb", "sm_egl"], writes=["S_sb"])
            tt("dve", S_sb[rows], S_sb[rows],
               pt[rows, :].rearrange("p (a d) -> p a d", d=128)[:, :, 64 * hp:64 * hp + 64], ALU.add,
               reads=["S_sb", "b3"], writes=["S_sb"])
            cp("act", S_bd[rows, :, 64 * hp:64 * hp + 64], S_sb[rows], reads=["S_sb"], writes=["S_bd"])
        if G7 < 9:
            return
        act(sq[:, 0:512], o_f[:].rearrange("p h d -> p (h d)"), AF.Square, reads=["o_f"], writes=["cb"])
        if G7 < 10:
            return
        red(sso, sq[:, 0:512].rearrange("p (h d) -> p h d", d=64), ALU.add, reads=["cb"], writes=["sm_sso"])
        if G7 < 11:
            return
        ts("dve", sso, sso, 1.0 / 64, EPS, ALU.mult, ALU.add, reads=["sm_sso"], writes=["sm_sso"])
        if G7 < 12:
            return
        act(sso, sso, AF.Sqrt, reads=["sm_sso"], writes=["sm_sso"])
        if G7 < 13:
            return
        recip(sso, sso, reads=["sm_sso"], writes=["sm_sso"])
        if G7 < 14:
            return
        tt("dve", o_f[:], o_f[:], bc8(sso), ALU.mult, reads=["o_f", "sm_sso"], writes=["o_f"])
        if G7 < 15:
            return
        tt("dve", o_f[:], o_f[:], gaB[:, None, :].to_broadcast([128, 8, 64]), ALU.mult, reads=["o_f", "gaB"],
           writes=["o_f"])
        if G7 < 16:
            return
        tt("dve", o_cat[:, 0:512], o_f[:].rearrange("p h d -> p (h d)"), zs[:], ALU.mult, reads=["o_f", "zs"],
           writes=["o_cat"])
        if G7 < 17:
            return
        if t == NT - 1:
            dma(dl_o, S_sb[:].rearrange("p a d -> p (a d)"), reads=["S_sb"])

    for t in range(NTT):
        rmsnorm_T(t)
        if t < NT:
            gdn_front(t)
        window_qkv(t)
        outputs_kv(t)
        if t < NT:
            ag = attn_prompt(t)
            ng = gdn_prompt(t)
            live = [ng, ag]
            while live:
                for g_ in list(live):
                    try:
                        next(g_)
                    except StopIteration:
                        live.remove(g_)
            attn_done(t)
            gdn_rec(t)
        if t < NT:
            dma(ocat_d[t * 128:(t + 1) * 128, :], o_cat[:], reads=["o_cat"])
        if t >= NT - 1:
            preconv_tm(t)

    R.emit(nc)
    es.close()
    es = ExitStack()

    def sb3(name, shape, dt=F32):
        return es.enter_context(nc.sbuf_tensor(name, list(shape), dt))

    xq = sb3("xq", [16, 1536])
    scv = sb3("scv", [16, 3, 1536])
    wcB = sb3("wcB", [16, 4, 1536])
    cacc = sb3("cacc", [16, 1536])
    ctmp = sb3("ctmp", [16, 1536])
    S128 = sb3("S128", [128, 64, 64])
    Kd = sb3("Kd", [128, 128, 64])
    Vd = sb3("Vd", [128, 128, 64])
    prod = sb3("prod", [128, 128, 64])
    v6 = sb3("v6", [128, 16, 64])
    s1 = sb3("s1", [128, 32])
    pj = sb3("pj", [128, 128])
    ocs = sb3("ocs", [128, 1024], BF)
    o16 = sb3("o16", [16, 1024])

    QC, KC, VC, Z, QR, KR, VW, KS, QS, VN, O, NUM, T1, GA, GB, T2 = [v6[:, i, :] for i in range(16)]
    (a_, b_, alog, dtb, la_, eg_, ssq, ssk, rq, rk, qk_, den, ps_, t_, rno, qkr) = [s1[:, i:i + 1] for i in range(16)]
    W3 = dict(reads=["v6", "s1"], writes=["v6"])
    W1 = dict(reads=["v6", "s1"], writes=["s1"])

    dma(xq[:], pre_s_d, writes=["xq"])
    dma(scv[:], sconv_d, writes=["scv"])
    dma(wcB[:].rearrange("p j c -> p (j c)"), wconv_flat_d.partition_broadcast(16), writes=["wcB"])
    dma(cv_s_o[:, 0:2, :], scv[:, 1:3, :], reads=["scv"])
    dma(cv_s_o[:, 2, :], xq[:], reads=["xq"])
    tt("dve", cacc[:], xq[:], wcB[:, 3, :], ALU.mult, reads=["xq", "wcB"], writes=["cacc"])
    for j in range(3):
        tt("pool", ctmp[:], scv[:, j, :], wcB[:, j, :], ALU.mult, reads=["scv", "wcB"], writes=["ctmp"])
        tt("dve", cacc[:], cacc[:], ctmp[:], ALU.add, reads=["cacc", "ctmp"], writes=["cacc"])
    act(cacc[:], cacc[:], AF.Silu, reads=["cacc"], writes=["cacc"])
    for j in range(3):
        dma(c3_d[j], cacc[:, j * 512:(j + 1) * 512], reads=["cacc"], writes=["c3_d"])
    for j, dst in enumerate((QC, KC, VC)):
        dma(dst, c3_d[j].rearrange("n (h d) -> (n h) d", d=64), reads=["c3_d"], writes=["v6"])
    dma(Z, z_s_d.rearrange("n (h d) -> (n h) d", d=64), writes=["v6"])
    dma(QR, qk_s_d[0].rearrange("n (h d) -> (n h) d", d=64), writes=["v6"])
    dma(KR, qk_s_d[1].rearrange("n (h d) -> (n h) d", d=64), writes=["v6"])
    dma(VW, v_s_d.rearrange("n (h d) -> (n h) d", d=64), writes=["v6"])
    dma(GA, g_a_d.partition_broadcast(128), writes=["v6"])
    dma(GB, g_b_d.partition_broadcast(128), writes=["v6"])
    dma(a_, ab_s_d[0].rearrange("n (h o) -> (n h) o", o=1), writes=["s1"])
    dma(b_, ab_s_d[1].rearrange("n (h o) -> (n h) o", o=1), writes=["s1"])
    dma(alog, alog128_d, writes=["s1"])
    dma(dtb, dtb128_d, writes=["s1"])
    dma(S128[:].rearrange("p a b -> p (a b)"), sdelta_d.rearrange("n h a b -> (n h) (a b)"), writes=["S128"])
    tt("dve", a_, a_, dtb, ALU.add, **W1)
    act(a_, a_, AF.Exp, **W1)
    act(a_, a_, AF.Ln, bias=1.0, **W1)
    act(alog, alog, AF.Exp, **W1)
    tt("dve", la_, a_, alog, ALU.mult, **W1)
    act(eg_, la_, AF.Exp, scale=-1.0, **W1)
    act(b_, b_, AF.Sigmoid, **W1)
    for src, ssx, rx in ((QC, ssq, rq), (KC, ssk, rk)):
        tt("dve", T1, src, src, ALU.mult, **W3)
        red(ssx, T1, ALU.add, **W1)
        ts("dve", ssx, ssx, EPS, None, ALU.add, **W1)
        act(ssx, ssx, AF.Sqrt, **W1)
        recip(rx, ssx, **W1)
        ts("dve", src, src, rx, None, ALU.mult, **W3)
    for src, dst in ((KC, KS), (QC, QS)):
        tt("dve", prod[:, 0:64, :], S128[:], src[:, :, None].to_broadcast([128, 64, 64]), ALU.mult,
           reads=["S128", "v6"], writes=["prod"])
        red(dst, prod[:, 0:64, :].rearrange("p a b -> p b a"), ALU.add, reads=["prod"], writes=["v6"])
    ts("dve", T1, KS, eg_, None, ALU.mult, **W3)
    tt("dve", T1, VC, T1, ALU.subtract, **W3)
    ts("dve", VN, T1, b_, None, ALU.mult, **W3)
    tt("dve", T1, QC, KC, ALU.mult, **W3)
    red(qk_, T1, ALU.add, **W1)
    ts("dve", O, QS, eg_, None, ALU.mult, **W3)
    stt(O, VN, qk_, O, ALU.mult, ALU.add, **W3)
    ts("dve", O, O, 0.125, None, ALU.mult, **W3)
    ts("dve", S128[:].rearrange("p a b -> p (a b)"), S128[:].rearrange("p a b -> p (a b)"), eg_, None, ALU.mult,
       reads=["S128", "s1"], writes=["S128"])
    tt("pool", prod[:, 0:64, :], KC[:, :, None].to_broadcast([128, 64, 64]), VN[:, None, :].to_broadcast([128, 64, 64]),
       ALU.mult, reads=["v6"], writes=["prod"])
    tt("dve", S128[:], S128[:], prod[:, 0:64, :], ALU.add, reads=["S128", "prod"], writes=["S128"])
    dma(dl_s_o.rearrange("n h a b -> (n h) (a b)"), S128[:].rearrange("p a b -> p (a b)"), reads=["S128"])
    def rms64(x_, g_vec, outp):
        tt("dve", T1, x_, x_, ALU.mult, **W3)
        red(rno, T1, ALU.add, **W1)
        ts("dve", rno, rno, 1.0 / 64, EPS, ALU.mult, ALU.add, **W1)
        act(rno, rno, AF.Sqrt, **W1)
        recip(rno, rno, **W1)
        ts("dve", x_, x_, rno, None, ALU.mult, **W3)
        tt("dve", outp, x_, g_vec, ALU.mult, **W3)
    rms64(O, GA, O)
    act(Z, Z, AF.Silu, **W3)
    tt("dve", O, O, Z, ALU.mult, **W3)
    dma(oab_d[0].rearrange("n (h d) -> (n h) d", d=64), O, reads=["v6"], writes=["oab_d"])
    R.op("pool", lambda e: e.memset(NUM, 0.0), writes=["v6n"])
    R.op("pool", lambda e: e.memset(den, 0.0), writes=["s1d"])
    for di, dil in enumerate((1, 4, 16)):
        st = 2048 - 128 * dil
        for n_ in range(16):
            dma(Kd[n_ * 8:(n_ + 1) * 8], ck_d[n_, st:2048:dil, :, :].rearrange("j h d -> h j d"), writes=["Kd"])
            dma(Vd[n_ * 8:(n_ + 1) * 8], cv_d[n_, st:2048:dil, :, :].rearrange("j h d -> h j d"), writes=["Vd"])
        tt("dve", prod[:], Kd[:], QR[:, None, :].to_broadcast([128, 128, 64]), ALU.mult, reads=["Kd", "v6"],
           writes=["prod"])
        red(pj[:], prod[:], ALU.add, reads=["prod"], writes=["pj"])
        act(pj[:], pj[:], AF.Exp, scale=0.125, reads=["pj"], writes=["pj"])
        red(t_, pj[:], ALU.add, reads=["pj"], writes=["s1t"])
        tt("dve", den, den, t_, ALU.add, reads=["s1d", "s1t"], writes=["s1d"])
        tt("pool", prod[:], Vd[:], pj[:, :, None].to_broadcast([128, 128, 64]), ALU.mult, reads=["Vd", "pj"],
           writes=["prod"])
        red(T2, prod[:].rearrange("p j d -> p d j"), ALU.add, reads=["prod"], writes=["v6t2"])
        tt("dve", NUM, NUM, T2, ALU.add, reads=["v6n", "v6t2"], writes=["v6n"])
    tt("dve", T1, QR, KR, ALU.mult, **W3)
    red(ps_, T1, ALU.add, **W1)
    act(ps_, ps_, AF.Exp, scale=0.125, **W1)
    ts("dve", ps_, ps_, 3.0, None, ALU.mult, **W1)
    tt("dve", den, den, ps_, ALU.add, reads=["s1", "s1d"], writes=["s1d"])
    stt(NUM, VW, ps_, NUM, ALU.mult, ALU.add, reads=["v6", "s1", "v6n"], writes=["v6n"])
    recip(den, den, reads=["s1d"], writes=["s1d"])
    ts("dve", NUM, NUM, den, None, ALU.mult, reads=["v6n", "s1d"], writes=["v6"])
    rms64(NUM, GB, NUM)
    dma(oab_d[1].rearrange("n (h d) -> (n h) d", d=64), NUM, reads=["v6"], writes=["oab_d"])
    dma(o16[:, 0:512], oab_d[0], reads=["oab_d"], writes=["o16"])
    dma(o16[:, 512:1024], oab_d[1], reads=["oab_d"], writes=["o16"])
    R.op("pool", lambda e: e.memset(ocs[:], 0.0), writes=["ocs"])
    cp("act", ocs[0:16, :], o16[:], reads=["o16", "ocs"], writes=["ocs"])
    dma(ocat_d[NT * 128:(NT + 1) * 128, :], ocs[:], reads=["ocs"])
    R.emit(nc)
    es.close()
    es = ExitStack()
    def sb2(name, shape, dt=F32):
        return es.enter_context(nc.sbuf_tensor(name, list(shape), dt))

    BS = 256
    NB = (2 * ROWS) // BS + 32
    SUB = BS // 128
    w_out = sb2("w_out_sb", [128, 8, 1024], BF)
    wpg = sb2("wpg_sb", [128, 8, 1024], BF)
    wpp = sb2("wpp_sb", [128, 2, 1024], BF)
    w_rt = sb2("w_rt_sb", [128, 8, 36])
    brtB = sb2("brtB", [128, 36])
    gffnB = sb2("gffnB", [128, 1024])
    gfinB = sb2("gfinB", [128, 1024])
    ident_f2 = sb2("ident_f2", [128, 128])
    ident_b2 = sb2("ident_b2", [128, 128], BF)
    triS = sb2("triS_sb", [128, 128])
    ones2 = sb2("ones2", [128, 128])
    B128 = sb2("B128", [128, NB])
    Wg = [sb2("Wg%d" % i, [128, 8, 512], BF) for i in range(2)]
    Wu = [sb2("Wu%d" % i, [128, 8, 512], BF) for i in range(2)]
    Wd = [sb2("Wd%d" % i, [128, 4, 1024], BF) for i in range(2)]
    h1t = sb2("h1t", [128, 1024])
    m_f = sb2("m_f", [128, 1024])
    m_b = sb2("m_b", [128, 1024], BF)
    sq2 = sb2("sq2", [128, 1024])
    oc_sb = sb2("oc_sb", [128, 1024], BF)
    ocT = sb2("ocT", [128, 8, 128], BF)
    xt2 = [sb2("xt2_%d" % i, [128, 1024]) for i in range(2)]
    p_sb = sb2("p_sb", [128, 256])
    p_b = sb2("p_b", [128, 256], BF)
    pT = sb2("pT", [128, 2, 128], BF)
    mTf = sb2("mTf", [128, 8, 128])
    h2b = sb2("h2b", [128, 1024], BF)
    h2T = sb2("h2T", [128, 8, 128], BF)
    sig = sb2("sig", [128, 1024])
    rs = sb2("rs", [128, 128])
    st2 = sb2("st2", [128, 4])
    OHs = sb2("OHs", [128, NTT, 64])
    rnk = sb2("rnk", [128, NTT, 2])
    gts = sb2("gts", [128, NTT, 2])
    dstF = sb2("dstF", [128, NTT, 2])
    dstI = sb2("dstI", [128, NTT, 2], I32)
    carry = sb2("carry", [128, 32])
    r32 = sb2("r32", [128, 8, 32])
    cmpb = sb2("cmpb", [128, NB, 32])
    E_f = sb2("E_f", [128, NB])
    idxW = sb2("idxW", [128, NB], I32)
    pidx = sb2("pidx_sb", [128, 1])
    xb = [sb2("xb%d" % i, [128, 1024], BF) for i in range(2)]
    xbT = [sb2("xbT%d" % i, [128, 8, 128], BF) for i in range(2)]
    sgt = [sb2("sgt%d" % i, [128, 512]) for i in range(2)]
    hb = [sb2("hb%d" % i, [128, 512], BF) for i in range(2)]
    hbT = [sb2("hbT%d" % i, [128, 4, 128], BF) for i in range(2)]
    yb = [sb2("yb%d" % i, [128, 1024]) for i in range(2)]
    yg = [sb2("yg%d" % i, [128, 1024]) for i in range(2)]
    zt = sb2("zt", [128, 1024], BF)
    bk = [pw[:, 0:512], pw[:, 512:1024], pw[:, 1024:1536], pt[:, :], ps2[:, 0:512], ps2[:, 512:1024],
          po2[:, 0:512], po2[:, 512:1024]]
    h1_d = nc.dram_tensor("h1_scr", [ROWS, 1024], F32, kind="Internal").ap()
    m_d = nc.dram_tensor("m_scr", [ROWS, 1024], BF, kind="Internal").ap()
    xbuf_d = nc.dram_tensor("xbuf_scr", [NB * BS, 1024], BF, kind="Internal").ap()
    ybuf_d = nc.dram_tensor("ybuf_scr", [NB * BS, 1024], F32, kind="Internal").ap()

    dma(ident_f2[:], ident_f_d, writes=["ident_f"])
    dma(ident_b2[:], ident_b_d, writes=["ident_b"])
    dma(triS[:], tris_d, writes=["triS"])
    dma(pidx[:], pidx_d, writes=["pidx"])
    dma(ones2[:], ones_d, writes=["ones2"])
    dma(B128[:], b128_d.partition_broadcast(128), writes=["B128"])
    dma(gffnB[:], g_ffn_d.partition_broadcast(128), writes=["gffnB"])
    dma(gfinB[:], g_fin_d.partition_broadcast(128), writes=["gfinB"])
    dma(brtB[:], b_rt_d.partition_broadcast(128), writes=["brtB"])
    dma(w_rt[:], w_rt_d.rearrange("(k p) n -> p k n", p=128), writes=["w_rt"])
    for k in range(8):
        dma(w_out[:, k, :], w_out_d.rearrange("(k p) n -> p k n", p=128)[:, k, :], writes=["w_out"], eng="pool")
        dma(wpg[:, k, :], wpg_d.rearrange("(k p) n -> p k n", p=128)[:, k, :], writes=["wpg"], eng="pool")
    for k in range(2):
        dma(wpp[:, k, :], wpp_d.rearrange("(k p) n -> p k n", p=128)[:, k, :], writes=["wpp"], eng="pool")
    R.op("pool", lambda e: e.memset(carry[:], 0.0), writes=["carry"])
    R.op("pool", lambda e: e.memset(zt[:], 0.0), writes=["zt"])
    for b in range(NB * SUB):
        dma(xbuf_d[b * 128:(b + 1) * 128, :], zt[:], reads=["zt"], writes=["xbuf_z"], eng="pool")

    def rms2(src, gB_, dst, tag):
        act(sq2[:], src, AF.Square, reads=[tag], writes=["sq2"])
        red(st2[:, 0:1], sq2[:], ALU.add, reads=["sq2"], writes=["st2"])
        ts("dve", st2[:, 0:1], st2[:, 0:1], 1.0 / D, EPS, ALU.mult, ALU.add, reads=["st2"], writes=["st2"])
        act(st2[:, 0:1], st2[:, 0:1], AF.Sqrt, reads=["st2"], writes=["st2"])
        recip(st2[:, 1:2], st2[:, 0:1], reads=["st2"], writes=["st2"])
        stt(dst, src, st2[:, 1:2], gB_[:], ALU.mult, ALU.mult, reads=[tag, "st2", "gffnB", "gfinB"], writes=["dst_" + tag])

    def routing(t):
        lg = rs[:, 0:36]
        tt("dve", lg, pt[:, 0:36], brtB[:], ALU.add, reads=["b3", "brtB"], writes=["rs"])
        gl4 = rs[:, 0:4]
        el = rs[:, 4:36].rearrange("p (g j) -> p g j", j=8)
        gmax, ngmax, gsum, pgrp = rs[:, 36:37], rs[:, 37:38], rs[:, 38:39], rs[:, 39:40]
        goh, gex = rs[:, 40:44], rs[:, 44:48]
        tmp = rs[:, 48:80].rearrange("p (g j) -> p g j", j=8)
        e_in, oh1, e2, oh2 = rs[:, 80:88], rs[:, 88:96], rs[:, 96:104], rs[:, 104:112]
        m1, m2, d21, w1, w2 = rs[:, 120:121], rs[:, 121:122], rs[:, 122:123], rs[:, 123:124], rs[:, 124:125]
        RW = dict(reads=["rs"], writes=["rs"])
        red(gmax, gl4, ALU.max, **RW)
        ts("dve", ngmax, gmax, -1.0, None, ALU.mult, **RW)
        ts("dve", goh, gl4, gmax, None, ALU.is_equal, **RW)
        act(gex, gl4, AF.Exp, bias=ngmax, **RW)
        red(gsum, gex, ALU.add, **RW)
        recip(pgrp, gsum, **RW)
        tt("dve", tmp, el, goh[:, :, None].to_broadcast([128, 4, 8]), ALU.mult, **RW)
        red(e_in, tmp.rearrange("p g j -> p j g"), ALU.add, **RW)
        red(m1, e_in, ALU.max, **RW)
        ts("dve", oh1, e_in, m1, None, ALU.is_equal, **RW)
        stt(e2, oh1, -1e30, e_in, ALU.mult, ALU.add, **RW)
        red(m2, e2, ALU.max, **RW)
        ts("dve", oh2, e2, m2, None, ALU.is_equal, **RW)
        tt("dve", d21, m2, m1, ALU.subtract, **RW)
        act(d21, d21, AF.Exp, **RW)
        ts("dve", w1, d21, 1.0, None, ALU.add, **RW)
        recip(w1, w1, **RW)
        tt("dve", w2, d21, w1, ALU.mult, **RW)
        tt("dve", gts[:, t, 0:1], w1, pgrp, ALU.mult, reads=["rs"], writes=["gts"])
        tt("dve", gts[:, t, 1:2], w2, pgrp, ALU.mult, reads=["rs"], writes=["gts"])
        for k_, ohk in ((0, oh1), (1, oh2)):
            tt("dve", OHs[:, t, 32 * k_:32 * k_ + 32].rearrange("p (g j) -> p g j", j=8),
               goh[:, :, None].to_broadcast([128, 4, 8]), ohk[:, None, :].to_broadcast([128, 4, 8]), ALU.mult,
               reads=["rs"], writes=["OHs"])
        oh12, cc, tm = r32[:, 0, :], r32[:, 1, :], r32[:, 2, :]
        tt("dve", oh12, OHs[:, t, 0:32], OHs[:, t, 32:64], ALU.add, reads=["OHs"], writes=["r32a"])
        mm(pt[:, 64:96], triS[:], oh12, True, True, reads=["triS", "r32a"], writes=["b3"])
        mm(pt[:, 96:128], ones2[:], oh12, True, True, reads=["ones2", "r32a"], writes=["b3"])
        tt("dve", cc, pt[:, 64:96], carry[:], ALU.add, reads=["b3", "carry"], writes=["r32b"])
        for k_ in range(2):
            tt("dve", tm, OHs[:, t, 32 * k_:32 * k_ + 32], cc, ALU.mult, reads=["OHs", "r32b"], writes=["r32c"])
            red(rnk[:, t, k_:k_ + 1], tm, ALU.add, reads=["r32c"], writes=["rnk"])
        tt("dve", carry[:], carry[:], pt[:, 96:128], ALU.add, reads=["carry", "b3"], writes=["carry"])

    pTb = pt[:, :].bitcast(BF)
    for t in range(NTT):
        xs = t % 2
        dma(oc_sb[:], ocat_d[t * 128:(t + 1) * 128, :], writes=["oc_sb"])
        dma(xt2[xs][:], x_d[t * 128:(t + 1) * 128, :], writes=["xt2_%d" % xs])
        for k in range(8):
            tr(pTb[:, k * 128:(k + 1) * 128], oc_sb[:, k * 128:(k + 1) * 128], ident_b2[:],
               reads=["oc_sb", "ident_b"], writes=["b3"])
        cp("act", ocT[:].rearrange("p k t -> p (k t)"), pTb, reads=["b3"], writes=["ocT"])
        for half in range(2):
            for k in range(8):
                mm(bk[4 + half], ocT[:, k, :], w_out[:, k, half * 512:(half + 1) * 512], k == 0, k == 7,
                   reads=["ocT", "w_out"], writes=["b%d" % (4 + half)])
        tt("dve", h1t[:], ps2[:, :], xt2[xs][:], ALU.add, reads=["b4", "b5", "xt2_%d" % xs], writes=["h1"])
        dma(h1_d[t * 128:(t + 1) * 128, :], h1t[:], reads=["h1"], writes=["h1_d"])
        rms2(h1t[:], gffnB, m_f[:], "h1")
        cp("act", m_b[:], m_f[:], reads=["dst_h1"], writes=["m_b"])
        dma(m_d[t * 128:(t + 1) * 128, :], m_b[:], reads=["m_b"], writes=["m_d"])
        for k in range(8):
            tr(po2[:, k * 128:(k + 1) * 128], m_f[:, k * 128:(k + 1) * 128], ident_f2[:],
               reads=["dst_h1", "ident_f"], writes=["b6", "b7"])
        cp("act", mTf[:].rearrange("p k t -> p (k t)"), po2[:, :], reads=["b6", "b7"], writes=["mTf"])
        for k in range(8):
            mm(pt[:, 0:36], mTf[:, k, :], w_rt[:, k, :], k == 0, k == 7, reads=["mTf", "w_rt"], writes=["b3"])
        routing(t)
    nblk, padded, pa, pb_, pstart = r32[:, 3, :], r32[:, 4, :], r32[:, 5, :], r32[:, 6, :], r32[:, 7, :]
    NJ = NB
    tt("dve", cmpb[:, 0:NJ, :].rearrange("p j e -> p e j"), carry[:, :, None].to_broadcast([128, 32, NJ]),
       B128[:, None, 0:NJ].to_broadcast([128, 32, NJ]), ALU.is_gt, reads=["carry", "B128"], writes=["cmpb"])
    red(nblk, cmpb[:, 0:NJ, :].rearrange("p j e -> p e j"), ALU.add, reads=["cmpb"], writes=["r32d"])
    ts("dve", padded, nblk, float(BS), None, ALU.mult, reads=["r32d"], writes=["r32e"])
    cp("pool", pa, padded, reads=["r32e"], writes=["r32f"])
    src_, dst_ = pa, pb_
    sn, dn = "r32f", "r32g"
    for s_ in (1, 2, 4, 8, 16):
        cp("pool", dst_[:, 0:s_], src_[:, 0:s_], reads=[sn], writes=[dn])
        tt("dve", dst_[:, s_:32], src_[:, s_:32], src_[:, 0:32 - s_], ALU.add, reads=[sn], writes=[dn])
        src_, dst_ = dst_, src_
        sn, dn = dn, sn
    pend = src_
    tt("dve", pstart, pend, padded, ALU.subtract, reads=[sn, "r32e"], writes=["r32h"])
    tt("dve", cmpb[:], pend[:, None, :].to_broadcast([128, NB, 32]), B128[:, :, None].to_broadcast([128, NB, 32]),
       ALU.is_le, reads=[sn, "B128", "cmpb"], writes=["cmpb"])
    red(E_f[:], cmpb[:], ALU.add, reads=["cmpb"], writes=["E_f"])
    ts("dve", E_f[:], E_f[:], 31.0, None, ALU.min, reads=["E_f"], writes=["E_f"])
    ts("dve", E_f[:], E_f[:], 128.0, pidx[:, 0:1], ALU.mult, ALU.add, reads=["E_f", "pidx"], writes=["E_f"])
    cp("dve", idxW[:], E_f[:], reads=["E_f"], writes=["idxW"])
    for t in range(NTT):
        for k_ in range(2):
            tm = r32[:, k_, :]
            tt("dve", tm, OHs[:, t, 32 * k_:32 * k_ + 32], pstart, ALU.mult, reads=["OHs", "r32h"], writes=["r32t%d" % k_])
            red(dstF[:, t, k_:k_ + 1], tm, ALU.add, reads=["r32t%d" % k_], writes=["dstF"])
        tt("dve", dstF[:, t, :], dstF[:, t, :], rnk[:, t, :], ALU.add, reads=["dstF", "rnk"], writes=["dstF"])
        cp("dve", dstI[:, t, :], dstF[:, t, :], reads=["dstF"], writes=["dstI"])
        xs = t % 2
        dma(xb[xs][:], m_d[t * 128:(t + 1) * 128, :], reads=["m_d"], writes=["xb%d" % xs])
        for k_ in range(2):
            R.op("pool", (lambda xs=xs, t=t, k_=k_: (lambda e: e.indirect_dma_start(
                out=xbuf_d[:, :], out_offset=bass.IndirectOffsetOnAxis(ap=dstI[:, t, k_:k_ + 1], axis=0),
                in_=xb[xs][:], in_offset=None)))(), reads=["xb%d" % xs, "dstI", "xbuf_z"], writes=["xbuf"], dma=True)
    def wload(b):
        s_ = b % 2
        for wt, wsrc, nm in ((Wg, wg_bf, "Wg"), (Wu, wu_bf, "Wu"), (Wd, wd_bf, "Wd")):
            R.op("pool", (lambda wt=wt, wsrc=wsrc, b=b, s_=s_: (lambda e: e.indirect_dma_start(
                out=wt[s_][:].rearrange("p a n -> p (a n)"), out_offset=None, in_=wsrc[:, :],
                in_offset=bass.IndirectOffsetOnAxis(ap=idxW[:, b:b + 1], axis=0))))(),
                reads=["idxW"], writes=["%s%d" % (nm, s_)], dma=True)

    def pbuf(q_):
        tb_ = pTb if q_ == 0 else pw[:, 1024:1536].bitcast(BF)
        tn = "b3" if q_ == 0 else "b2"
        gbk, gn = (bk[4], "b4") if q_ == 0 else (bk[0], "b0")
        ubk, un = (bk[5], "b5") if q_ == 0 else (bk[1], "b1")
        return tb_, tn, gbk, gn, ubk, un

    def stage1(i):
        b, sub = i // SUB, i % SUB
        s_, q_ = b % 2, i % 2
        r0 = b * BS + sub * 128
        tb_, tn, gbk, gn, ubk, un = pbuf(q_)
        if sub == 0:
            wload(b)
        dma(xb[q_][:], xbuf_d[r0:r0 + 128, :], reads=["xbuf"], writes=["xb%d" % q_])
        for k in range(8):
            tr(tb_[:, k * 128:(k + 1) * 128], xb[q_][:, k * 128:(k + 1) * 128], ident_b2[:],
               reads=["xb%d" % q_, "ident_b"], writes=[tn])
        cp("act", xbT[q_][:].rearrange("p k t -> p (k t)"), tb_, reads=[tn], writes=["xbT%d" % q_])
        for k in range(8):
            mm(gbk, xbT[q_][:, k, :], Wg[s_][:, k, :], k == 0, k == 7, reads=["xbT%d" % q_, "Wg%d" % s_], writes=[gn])
        for k in range(8):
            mm(ubk, xbT[q_][:, k, :], Wu[s_][:, k, :], k == 0, k == 7, reads=["xbT%d" % q_, "Wu%d" % s_], writes=[un])
        act(sgt[q_][:], gbk, AF.Silu, reads=[gn], writes=["sgt%d" % q_])
        tt("dve", hb[q_][:], sgt[q_][:], ubk, ALU.mult, reads=["sgt%d" % q_, un], writes=["hb%d" % q_])

    def stage2(i):
        b, sub = i // SUB, i % SUB
        s_, q_ = b % 2, i % 2
        r0 = b * BS + sub * 128
        tb_, tn, gbk, gn, ubk, un = pbuf(q_)
        for c in range(4):
            tr(tb_[:, c * 128:(c + 1) * 128], hb[q_][:, c * 128:(c + 1) * 128], ident_b2[:],
               reads=["hb%d" % q_, "ident_b"], writes=[tn])
        cp("act", hbT[q_][:].rearrange("p c t -> p (c t)"), tb_[:, 0:512], reads=[tn], writes=["hbT%d" % q_])
        for half in range(2):
            for c in range(4):
                mm(bk[6 + half], hbT[q_][:, c, :], Wd[s_][:, c, half * 512:(half + 1) * 512], c == 0, c == 3,
                   reads=["hbT%d" % q_, "Wd%d" % s_], writes=["b%d" % (6 + half)])
        cp("act", yb[q_][:], po2[:, :], reads=["b6", "b7"], writes=["yb%d" % q_])
        dma(ybuf_d[r0:r0 + 128, :], yb[q_][:], reads=["yb%d" % q_], writes=["ybuf"])

    NSUBT = NB * SUB
    stage1(0)
    for i in range(NSUBT):
        if i + 1 < NSUBT:
            stage1(i + 1)
        stage2(i)
    for t in range(NTT):
        xs = t % 2
        dma(h1t[:], h1_d[t * 128:(t + 1) * 128, :], reads=["h1_d"], writes=["h1"])
        dma(p_sb[:], p_d[t * 128:(t + 1) * 128, :], writes=["p_sb"])
        for k_ in range(2):
            R.op("pool", (lambda t=t, k_=k_: (lambda e: e.indirect_dma_start(
                out=yg[k_][:], out_offset=None, in_=ybuf_d[:, :],
                in_offset=bass.IndirectOffsetOnAxis(ap=dstI[:, t, k_:k_ + 1], axis=0))))(),
                reads=["ybuf", "dstI"], writes=["yg%d" % k_], dma=True)
            stt(h1t[:], yg[k_][:], gts[:, t, k_:k_ + 1], h1t[:], ALU.mult, ALU.add, reads=["yg%d" % k_, "gts", "h1"],
                writes=["h1"])
        cp("act", h2b[:], h1t[:], reads=["h1"], writes=["h2b"])
        for k in range(8):
            tr(pTb[:, k * 128:(k + 1) * 128], h2b[:, k * 128:(k + 1) * 128], ident_b2[:], reads=["h2b", "ident_b"],
               writes=["b3"])
        cp("act", h2T[:].rearrange("p k t -> p (k t)"), pTb, reads=["b3"], writes=["h2T"])
        for half in range(2):
            for k in range(8):
                mm(bk[4 + half], h2T[:, k, :], wpg[:, k, half * 512:(half + 1) * 512], k == 0, k == 7,
                   reads=["h2T", "wpg"], writes=["b%d" % (4 + half)])
        act(sig[:], ps2[:, :], AF.Sigmoid, reads=["b4", "b5"], writes=["sig"])
        cp("act", p_b[:], p_sb[:], reads=["p_sb"], writes=["p_b"])
        for k in range(2):
            tr(pTb[:, k * 128:(k + 1) * 128], p_b[:, k * 128:(k + 1) * 128], ident_b2[:], reads=["p_b", "ident_b"],
               writes=["b3"])
        cp("act", pT[:].rearrange("p k t -> p (k t)"), pTb[:, 0:256], reads=["b3"], writes=["pT"])
        for half in range(2):
            for k in range(2):
                mm(bk[6 + half], pT[:, k, :], wpp[:, k, half * 512:(half + 1) * 512], k == 0, k == 1,
                   reads=["pT", "wpp"], writes=["b%d" % (6 + half)])
        tt("dve", sig[:], sig[:], po2[:, :], ALU.mult, reads=["sig", "b6", "b7"], writes=["sig"])
        tt("dve", h1t[:], h1t[:], sig[:], ALU.add, reads=["h1", "sig"], writes=["h1"])
        rms2(h1t[:], gfinB, m_f[:], "h1")
        dma(y_d[t * 128:(t + 1) * 128, :], m_f[:], reads=["dst_h1"])
    R.emit(nc)
    es.close()
    esg.close()
    return nc


_CACHE = {}


def _cmask():
    k = np.arange(128)[:, None, None]
    dl = np.arange(NWIN)[None, :, None]
    q = np.arange(128)[None, None, :]
    d = 128 * dl + q - k
    c = ((d <= 128).astype(np.float32) + ((d % 4 == 0) & (d <= 512)) + ((d % 16 == 0) & (d <= 2048))) * (d >= 0)
    return c.reshape(128, NWIN * 128).astype(ml_dtypes.bfloat16)


def _consts(NT):
    NTT = NT + 1
    pos = np.concatenate([np.arange(NT * 128, dtype=np.float32), np.full(128, 8192.0, np.float32)])
    half = 8
    inv = (np.float32(500000.0) ** (-np.arange(half, dtype=np.float32) * np.float32(2.0 / 16))).astype(np.float32)
    ang = (pos[:, None] * inv[None, :]).astype(np.float32)
    return {
        "cos_t": np.cos(ang).astype(np.float32),
        "sin_t": np.sin(ang).astype(np.float32),
        "ident_f": np.eye(128, dtype=np.float32),
        "ident_b": np.eye(128).astype(ml_dtypes.bfloat16),
        "cmask": _cmask(),
        "triF": np.triu(np.ones((128, 128), np.float32)),
        "onesF": np.ones((128, 128), np.float32),
        "triS": np.triu(np.ones((128, 128), np.float32), 1),
        "pidx": np.arange(128, dtype=np.float32)[:, None],
        "b128": (256.0 * np.arange((2 * (NT + 1) * 128) // 256 + 32, dtype=np.float32))[None, :],
        "triN": -np.triu(np.ones((128, 128), np.float32)),
        "maskUi": np.where(np.arange(128)[None, :] >= np.arange(128)[:, None], 0.0, -30000.0).astype(np.float32),
        "maskUs": np.where(np.arange(128)[None, :] > np.arange(128)[:, None], 0.0, -30000.0).astype(np.float32),
        "maskLs": np.where(np.arange(128)[None, :] < np.arange(128)[:, None], 0.0, -30000.0).astype(np.float32),
    }


def run(inputs, NT, n_cores, KEEP):
    key = (NT, KEEP)
    if key not in _CACHE:
        _CACHE[key] = build(NT, KEEP)
    nc = _CACHE[key]
    S = NT * 128
    consts = _consts(NT)
    in_maps = []
    for c in range(n_cores):
        xs = np.zeros((128, D), np.float32)
        xs[0:16] = inputs["x_sample"][16 * c:16 * c + 16, 0]
        x = np.concatenate([inputs["x_prompt"][c], xs], axis=0)
        m = {"x": np.ascontiguousarray(x),
             "w_in": np.ascontiguousarray(inputs["w_in"][0]),
             "g_attn": np.ascontiguousarray(inputs["g_attn_norm"][0][None, :]),
             "g_b": np.ascontiguousarray(inputs["g_b_out"][0][None, :]),
             "g_a": np.ascontiguousarray(inputs["g_a_out"][0][None, :]),
             "w_out": np.ascontiguousarray(inputs["w_out"][0]),
             "state_conv": np.ascontiguousarray(inputs["state_conv"][0, 16 * c:16 * c + 16]),
             "state_delta": np.ascontiguousarray(inputs["state_delta"][0, 16 * c:16 * c + 16]),
             "cache_k": np.ascontiguousarray(inputs["cache_win_k"][0, 16 * c:16 * c + 16]),
             "cache_v": np.ascontiguousarray(inputs["cache_win_v"][0, 16 * c:16 * c + 16]),
             "wconv_flat": np.ascontiguousarray(inputs["w_conv"][0].reshape(1, -1)),
             "alog128": np.ascontiguousarray(np.tile(inputs["a_log"][0], 16)[:, None]),
             "dtb128": np.ascontiguousarray(np.tile(inputs["dt_bias"][0], 16)[:, None]),
             "g_ffn": np.ascontiguousarray(inputs["g_ffn_norm"][0][None, :]),
             "g_fin": np.ascontiguousarray(inputs["g_final"][None, :]),
             "w_rt": np.ascontiguousarray(np.concatenate(
                 [inputs["w_router_group"][0], inputs["w_router_expert"][0].reshape(D, 32)], axis=1)),
             "b_rt": np.ascontiguousarray(np.concatenate(
                 [inputs["b_router_group"][0], inputs["b_router_expert"][0].reshape(32)])[None, :]),
             "w_eg": np.ascontiguousarray(inputs["w_exp_gate"][0]),
             "w_eu": np.ascontiguousarray(inputs["w_exp_up"][0]),
             "w_ed": np.ascontiguousarray(inputs["w_exp_down"][0]),
             "w_pg": np.ascontiguousarray(inputs["w_ple_gate"][0]),
             "w_pp": np.ascontiguousarray(inputs["w_ple_proj"][0]),
             "p_all": np.ascontiguousarray(np.concatenate(
                 [inputs["p_prompt"][0, c], np.concatenate([inputs["p_sample"][0, 16 * c:16 * c + 16, 0],
                                                            np.zeros((112, 256), np.float32)])], axis=0)),
             "a_log": np.ascontiguousarray(inputs["a_log"][0][None, :]),
             "dt_bias": np.ascontiguousarray(inputs["dt_bias"][0][None, :]),
             "wconvT": np.ascontiguousarray(
                 inputs["w_conv"][0].T.reshape(12, 128, 4).transpose(1, 0, 2).reshape(128, 48))}
        m.update(consts)
        in_maps.append(m)
    res = run_bass_kernel_spmd(nc, in_maps, core_ids=list(range(n_cores)))
    return res.results


def kernel(**inputs):
    inputs = {k: np.asarray(v) for k, v in inputs.items()}
    NT, KEEP, n = 64, 2048, 8
    r = run(inputs, NT, n, KEEP)
    NP, S = 8, 8192
    y_p = np.stack([r[c]["y"][:S] for c in range(n)])
    y_s = np.concatenate([r[c]["y"][S:S + 16] for c in range(n)])[:, None, :]
    wk_p = np.stack([r[c]["wk_p"].reshape(KEEP, 8, 64) for c in range(n)])[None]
    wv_p = np.stack([r[c]["wv_p"].reshape(KEEP, 8, 64) for c in range(n)])[None]
    cv_p = np.stack([r[c]["cv_p"] for c in range(n)])[None]
    dl_p = np.stack([r[c]["dl_p"].reshape(2, 64, 4, 64).transpose(2, 0, 1, 3).reshape(8, 64, 64) for c in range(n)])[None]
    wk_s = np.concatenate([r[c]["wk_s"].reshape(16, 1, 8, 64) for c in range(n)])[None]
    wv_s = np.concatenate([r[c]["wv_s"].reshape(16, 1, 8, 64) for c in range(n)])[None]
    cv_s = np.concatenate([r[c]["cv_s"] for c in range(n)])[None]
    dl_s = np.concatenate([r[c]["dl_s"] for c in range(n)])[None]
    return (y_p.astype(np.float32), y_s.astype(np.float32), wk_p.astype(np.float32), wv_p.astype(np.float32), cv_p.astype(np.float32), dl_p.astype(np.float32),
            wk_s.astype(np.float32), wv_s.astype(np.float32), cv_s.astype(np.float32), dl_s.astype(np.float32))
```
